# Optimizing a Trainium2 kernel written in Bass

```python
import math
import jax, jax.numpy as jnp
from jax import lax
import numpy as np

D_MODEL = 1024
BATCH = 16
SEQ = 2048
DEPTH = 1

RET_HEADS = 4
RET_DK = 128
RET_DV = 256
RET_CHUNK = 128
ROPE_BASE = 10000.0
ATT_HEADS = 8
ATT_DH = 64
DILATED_PATTERNS = ((128, 1), (512, 4), (2048, 16))
NUM_BUCKETS = 32
MAX_DISTANCE = 1024
N_GROUPS = 4
EXPERTS_PER_GROUP = 4
N_EXPERTS = N_GROUPS * EXPERTS_PER_GROUP
TOP_K = 2
D_EXPERT = 512
MOE_BLOCK = 128
DEEPNORM_ALPHA = (2.0 * DEPTH) ** 0.25
DEEPNORM_BETA = (8.0 * DEPTH) ** -0.25
LN_EPS = 1e-5
NEG_INF = -1e30

RET_QK_W = RET_HEADS * RET_DK
RET_V_W = RET_HEADS * RET_DV
ATT_W = ATT_HEADS * ATT_DH
IN_SPLITS = (RET_QK_W, RET_QK_W, RET_V_W, RET_V_W, ATT_W, ATT_W, ATT_W, D_MODEL, D_MODEL)
IN_OFFSETS = tuple(int(v) for v in np.cumsum((0,) + IN_SPLITS))
IN_COLS = IN_OFFSETS[-1]
SPLIT_POINTS = list(IN_OFFSETS[1:-1])

kernel_name = 'hybrid_retention_dilated_hmoe_block'


def layer_norm(x, g, b):
    xf = x.astype(jnp.float32)
    mu = jnp.mean(xf, axis=-1, keepdims=True)
    var = jnp.mean(jnp.square(xf - mu), axis=-1, keepdims=True)
    y = (xf - mu) * lax.rsqrt(var + LN_EPS) * g.astype(jnp.float32) + b.astype(jnp.float32)
    return y.astype(x.dtype)


def rotary(t):
    s, dh = t.shape[1], t.shape[-1]
    half = dh // 2
    inv = ROPE_BASE ** (-jnp.arange(half, dtype=jnp.float32) / half)
    ang = jnp.arange(s, dtype=jnp.float32)[:, None] * inv[None, :]
    cos = jnp.cos(ang)[None, :, None, :]
    sin = jnp.sin(ang)[None, :, None, :]
    t1 = t[..., :half].astype(jnp.float32)
    t2 = t[..., half:].astype(jnp.float32)
    return jnp.concatenate([t1 * cos - t2 * sin, t1 * sin + t2 * cos], axis=-1).astype(t.dtype)


def retention_chunkwise(q, k, v, log_gamma):
    b, h, s, dk = q.shape
    dv = v.shape[-1]
    c = RET_CHUNK
    n = s // c
    qc = q.reshape(b, h, n, c, dk)
    kc = k.reshape(b, h, n, c, dk)
    vc = v.reshape(b, h, n, c, dv)
    pos = jnp.arange(c, dtype=jnp.float32)
    diff = pos[:, None] - pos[None, :]
    lg = log_gamma[:, None, None]
    decay = jnp.where(diff >= 0, jnp.exp(lg * jnp.maximum(diff, 0.0)), 0.0).astype(q.dtype)
    scores = jnp.einsum('bhnqd,bhnkd->bhnqk', qc, kc) * decay[None, :, None]
    inner = jnp.einsum('bhnqk,bhnkv->bhnqv', scores, vc)
    zeta = jnp.exp(log_gamma[:, None] * (c - 1 - pos)).astype(q.dtype)
    xi = jnp.exp(log_gamma[:, None] * (pos + 1)).astype(q.dtype)
    chunk_decay = jnp.exp(log_gamma * c).astype(q.dtype)[None, :, None, None]
    kv = jnp.einsum('bhnkd,hk,bhnkv->nbhdv', kc, zeta, vc)

    def step(state, kv_n):
        return state * chunk_decay + kv_n, state

    _, prev = lax.scan(step, jnp.zeros((b, h, dk, dv), q.dtype), kv)
    cross = jnp.einsum('bhnqd,nbhdv,hq->bhnqv', qc, prev, xi)
    return (inner + cross).reshape(b, h, s, dv)


def head_group_norm(o, g):
    of = o.astype(jnp.float32)
    mu = jnp.mean(of, axis=-1, keepdims=True)
    var = jnp.mean(jnp.square(of - mu), axis=-1, keepdims=True)
    y = ((of - mu) * lax.rsqrt(var + LN_EPS)).reshape(o.shape[0], o.shape[1], -1)
    return (y * g.astype(jnp.float32)).astype(o.dtype)


def t5_bucket(rel):
    half = NUM_BUCKETS // 2
    max_exact = half // 2
    side = jnp.where(rel > 0, half, 0)
    n = jnp.abs(rel)
    large = max_exact + (jnp.log(jnp.maximum(n, 1).astype(jnp.float32) / max_exact)
                         / math.log(MAX_DISTANCE / max_exact) * (half - max_exact)).astype(jnp.int32)
    large = jnp.minimum(large, half - 1)
    return side + jnp.where(n < max_exact, n, large)


def dilated_pattern(q, k, v, rel_bias, window, dil):
    b, s, h, dh = q.shape
    radius = window // (2 * dil)
    blk = radius
    n = s // dil
    nb = -(-n // blk)
    pad_n = nb * blk - n

    def by_stride(t):
        return t.reshape(b, n, dil, h, dh).transpose(0, 2, 1, 3, 4)

    qs, ks, vs = by_stride(q), by_stride(k), by_stride(v)
    qb = jnp.pad(qs, ((0, 0), (0, 0), (0, pad_n), (0, 0), (0, 0))).reshape(b, dil, nb, blk, h, dh)

    def band(t):
        tp = jnp.pad(t, ((0, 0), (0, 0), (blk, blk + pad_n), (0, 0), (0, 0)))
        blocks = tp.reshape(b, dil, nb + 2, blk, h, dh)
        return jnp.concatenate([blocks[:, :, :-2], blocks[:, :, 1:-1], blocks[:, :, 2:]], axis=3)

    kb, vb = band(ks), band(vs)
    qi = np.arange(blk)
    kk = np.arange(3 * blk)
    delta = kk[None, :] - blk - qi[:, None]
    in_window = np.abs(delta) <= radius
    key_sub = np.arange(nb)[:, None] * blk + kk[None, :] - blk
    valid = (key_sub >= 0) & (key_sub < n)
    mask = jnp.asarray(in_window[None, :, :] & valid[:, None, :])
    bias = rel_bias[t5_bucket(jnp.asarray(delta * dil))].astype(jnp.float32).transpose(2, 0, 1)

    logits = jnp.einsum('brnqhe,brnkhe->brnhqk', qb, kb).astype(jnp.float32) + bias[None, None, None]
    logits = jnp.where(mask[None, None, :, None], logits, NEG_INF)
    m = jnp.max(logits, axis=-1, keepdims=True)
    p = jnp.exp(logits - m)
    denom = jnp.sum(p, axis=-1, keepdims=True)
    o = jnp.einsum('brnhqk,brnkhe->brnhqe', p.astype(v.dtype), vb) / denom.astype(v.dtype)
    lse = (m + jnp.log(denom))[..., 0]
    o = o.transpose(0, 2, 4, 1, 3, 5).reshape(b, nb * blk, dil, h, dh)[:, :n].reshape(b, s, h, dh)
    lse = lse.transpose(0, 2, 4, 1, 3).reshape(b, nb * blk, dil, h)[:, :n].reshape(b, s, h)
    return o, lse


def dilated_attention(q, k, v, rel_bias):
    outs, lses = [], []
    for window, dil in DILATED_PATTERNS:
        o, lse = dilated_pattern(q, k, v, rel_bias, window, dil)
        outs.append(o)
        lses.append(lse)
    w = jax.nn.softmax(jnp.stack(lses, axis=0), axis=0)
    return jnp.sum(w[..., None].astype(q.dtype) * jnp.stack(outs, axis=0), axis=0)


def token_mixers(x, w_in, ret_decay_fwd, ret_decay_bwd, ret_gn_g, w_proj_ret, rel_bias, w_proj_attn, w_out):
    b, s, _ = x.shape
    hcat = x @ w_in
    q_r, k_r, v_r, g_r, q_a, k_a, v_a, gate_r, gate_a = jnp.split(hcat, SPLIT_POINTS, axis=-1)
    q_r = rotary(q_r.reshape(b, s, RET_HEADS, RET_DK))
    k_r = rotary(k_r.reshape(b, s, RET_HEADS, RET_DK)) * (RET_DK ** -0.5)
    v_r = v_r.reshape(b, s, RET_HEADS, RET_DV)
    qh, kh, vh = (t.transpose(0, 2, 1, 3) for t in (q_r, k_r, v_r))
    lg_f = jax.nn.log_sigmoid(ret_decay_fwd.astype(jnp.float32))
    lg_b = jax.nn.log_sigmoid(ret_decay_bwd.astype(jnp.float32))
    o_f = retention_chunkwise(qh, kh, vh, lg_f)
    o_b = jnp.flip(retention_chunkwise(jnp.flip(qh, 2), jnp.flip(kh, 2), jnp.flip(vh, 2), lg_b), 2)
    o_r = head_group_norm((o_f + o_b).transpose(0, 2, 1, 3), ret_gn_g)
    y_r = (jax.nn.silu(g_r) * o_r) @ w_proj_ret
    q_a = q_a.reshape(b, s, ATT_HEADS, ATT_DH) * (ATT_DH ** -0.5)
    k_a = k_a.reshape(b, s, ATT_HEADS, ATT_DH)
    v_a = v_a.reshape(b, s, ATT_HEADS, ATT_DH)
    y_a = dilated_attention(q_a, k_a, v_a, rel_bias).reshape(b, s, ATT_W) @ w_proj_attn
    merged = jax.nn.sigmoid(gate_r) * y_r + jax.nn.sigmoid(gate_a) * y_a
    return merged @ w_out


def hierarchical_moe(x, router_w_group, router_b_group, router_w_expert, router_b_expert, w_gate, w_up, w_down):
    b, s, d = x.shape
    t = b * s
    xt = x.reshape(t, d)
    g_logits = (xt @ router_w_group).astype(jnp.float32) + router_b_group.astype(jnp.float32)
    g_prob = jax.nn.softmax(g_logits, axis=-1)
    g_idx = jnp.argmax(g_logits, axis=-1)
    g_w = jnp.take_along_axis(g_prob, g_idx[:, None], axis=-1)
    e_logits = ((xt @ router_w_expert).astype(jnp.float32) + router_b_expert.astype(jnp.float32)).reshape(t, N_GROUPS, EXPERTS_PER_GROUP)
    e_logits = jnp.take_along_axis(e_logits, g_idx[:, None, None], axis=1)[:, 0]
    top_v, top_i = lax.top_k(e_logits, TOP_K)
    weights = g_w * jax.nn.softmax(top_v, axis=-1)
    expert_id = g_idx[:, None].astype(jnp.int32) * EXPERTS_PER_GROUP + top_i.astype(jnp.int32)
    a = t * TOP_K
    flat_e = expert_id.reshape(a)
    flat_w = weights.reshape(a)
    order = jnp.argsort(flat_e)
    e_sorted = flat_e[order]
    tok_sorted = (order // TOP_K).astype(jnp.int32)
    counts = jnp.zeros((N_EXPERTS,), jnp.int32).at[flat_e].add(1)
    padded = (counts + MOE_BLOCK - 1) // MOE_BLOCK * MOE_BLOCK
    start = jnp.cumsum(counts) - counts
    ends = jnp.cumsum(padded)
    pstart = ends - padded
    dest = pstart[e_sorted] + jnp.arange(a, dtype=jnp.int32) - start[e_sorted]
    n_blocks = -(-a // MOE_BLOCK) + N_EXPERTS
    slot_tok = jnp.full((n_blocks * MOE_BLOCK,), t, jnp.int32).at[dest].set(tok_sorted)
    block_start = jnp.arange(n_blocks, dtype=jnp.int32) * MOE_BLOCK
    block_expert = jnp.minimum(jnp.sum(ends[None, :] <= block_start[:, None], axis=1), N_EXPERTS - 1).astype(jnp.int32)
    x_pad = jnp.concatenate([xt, jnp.zeros((1, d), xt.dtype)], axis=0)
    xb = x_pad[slot_tok].reshape(n_blocks, MOE_BLOCK, d)

    def expert_block(args):
        xblk, e = args
        hid = jax.nn.silu(xblk @ w_gate[e]) * (xblk @ w_up[e])
        return hid @ w_down[e]

    yb = lax.map(expert_block, (xb, block_expert)).reshape(n_blocks * MOE_BLOCK, d)
    y_assign = yb[dest] * flat_w[order][:, None].astype(yb.dtype)
    out = jnp.zeros((t, d), yb.dtype).at[tok_sorted].add(y_assign)
    return out.reshape(b, s, d)


def setup_inputs(seed: int = 0) -> dict:
    key = jax.random.key(seed)
    ks = jax.random.split(key, 24)
    L = DEPTH

    def nrm(k, shape, scale):
        return jax.random.normal(k, shape, jnp.float32) * scale

    x = nrm(ks[0], (BATCH, SEQ, D_MODEL), 1.0)
    col = jnp.arange(IN_COLS)
    v_cols = ((col >= IN_OFFSETS[2]) & (col < IN_OFFSETS[3])) | ((col >= IN_OFFSETS[6]) & (col < IN_OFFSETS[7]))
    w_in = nrm(ks[1], (L, D_MODEL, IN_COLS), D_MODEL ** -0.5) * jnp.where(v_cols, DEEPNORM_BETA, 1.0)
    gamma = 1.0 - jnp.exp(jnp.linspace(math.log(1.0 / 32), math.log(1.0 / 512), RET_HEADS))
    logit = jnp.log(gamma) - jnp.log1p(-gamma)
    ret_decay_fwd = logit[None, :] + nrm(ks[2], (L, RET_HEADS), 0.05)
    ret_decay_bwd = logit[None, :] + nrm(ks[3], (L, RET_HEADS), 0.05)
    ret_gn_g = 1.0 + nrm(ks[4], (L, RET_V_W), 0.02)
    w_proj_ret = nrm(ks[5], (L, RET_V_W, D_MODEL), RET_V_W ** -0.5 * DEEPNORM_BETA)
    rel_bias = nrm(ks[6], (NUM_BUCKETS, ATT_HEADS), 0.5)
    w_proj_attn = nrm(ks[7], (L, ATT_W, D_MODEL), ATT_W ** -0.5 * DEEPNORM_BETA)
    w_out = nrm(ks[8], (L, D_MODEL, D_MODEL), D_MODEL ** -0.5 * DEEPNORM_BETA)
    ln1_g = 1.0 + nrm(ks[9], (L, D_MODEL), 0.02)
    ln1_b = nrm(ks[10], (L, D_MODEL), 0.02)
    router_w_group = nrm(ks[11], (L, D_MODEL, N_GROUPS), D_MODEL ** -0.5)
    router_b_group = nrm(ks[12], (L, N_GROUPS), 0.01)
    router_w_expert = nrm(ks[13], (L, D_MODEL, N_EXPERTS), D_MODEL ** -0.5)
    router_b_expert = nrm(ks[14], (L, N_EXPERTS), 0.01)
    w_gate = nrm(ks[15], (L, N_EXPERTS, D_MODEL, D_EXPERT), D_MODEL ** -0.5)
    w_up = nrm(ks[16], (L, N_EXPERTS, D_MODEL, D_EXPERT), D_MODEL ** -0.5)
    w_down = nrm(ks[17], (L, N_EXPERTS, D_EXPERT, D_MODEL), D_EXPERT ** -0.5 * DEEPNORM_BETA)
    ln2_g = 1.0 + nrm(ks[18], (L, D_MODEL), 0.02)
    ln2_b = nrm(ks[19], (L, D_MODEL), 0.02)
    return {'x': x, 'w_in': w_in, 'ret_decay_fwd': ret_decay_fwd, 'ret_decay_bwd': ret_decay_bwd,
            'ret_gn_g': ret_gn_g, 'w_proj_ret': w_proj_ret, 'rel_bias': rel_bias, 'w_proj_attn': w_proj_attn,
            'w_out': w_out, 'ln1_g': ln1_g, 'ln1_b': ln1_b, 'router_w_group': router_w_group,
            'router_b_group': router_b_group, 'router_w_expert': router_w_expert, 'router_b_expert': router_b_expert,
            'w_gate': w_gate, 'w_up': w_up, 'w_down': w_down, 'ln2_g': ln2_g, 'ln2_b': ln2_b}


def reference(x, w_in, ret_decay_fwd, ret_decay_bwd, ret_gn_g, w_proj_ret, rel_bias, w_proj_attn, w_out,
              ln1_g, ln1_b, router_w_group, router_b_group, router_w_expert, router_b_expert,
              w_gate, w_up, w_down, ln2_g, ln2_b):
    for l in range(DEPTH):
        mix = token_mixers(x, w_in[l], ret_decay_fwd[l], ret_decay_bwd[l], ret_gn_g[l], w_proj_ret[l],
                           rel_bias, w_proj_attn[l], w_out[l])
        x = layer_norm(DEEPNORM_ALPHA * x + mix, ln1_g[l], ln1_b[l])
        ffn = hierarchical_moe(x, router_w_group[l], router_b_group[l], router_w_expert[l], router_b_expert[l],
                               w_gate[l], w_up[l], w_down[l])
        x = layer_norm(DEEPNORM_ALPHA * x + ffn, ln2_g[l], ln2_b[l])
    return x
```

```python
import math
from contextlib import ExitStack
import numpy as np
import concourse.bass as bass
import concourse.mybir as mybir
from concourse.bass_utils import run_bass_kernel_spmd

F32 = mybir.dt.float32
BF16 = mybir.dt.bfloat16
U8 = mybir.dt.uint8
AF = mybir.ActivationFunctionType
ALU = mybir.AluOpType
AX = mybir.AxisListType

NCORES = 8
S = 2048
D = 1024
NSEQ = 2
ALPHA = 2.0 ** 0.25
EPS = 1e-5
CK = 128.0 ** -0.5
LNCK = math.log(CK)
NEG = -1e30
PATTERNS = (1, 4, 16)
DSIZE = {F32: 4, BF16: 2, U8: 1}
DBG = False
LAST = {}


class Buf:
    __slots__ = ("w", "r", "excl")

    def __init__(self, excl=False):
        self.w = {}
        self.r = {}
        self.excl = excl


class DSem:
    def __init__(self, sem):
        self.sem = sem
        self.count = 0


class Op:
    __slots__ = ("eng", "fn", "deps", "signaled", "count", "dsem", "dval", "id")


ENGS = ("sync", "act", "dve", "pool", "pe")


class Prog:
    def __init__(self):
        self.ops = {e: [] for e in ENGS}
        self.last = {}
        self.n = 0

    def add(self, eng, fn, reads=(), writes=(), dsem=None, extra=()):
        o = Op()
        o.eng = eng
        o.fn = fn
        o.signaled = False
        o.count = None
        o.dsem = dsem
        o.dval = None
        o.id = self.n
        self.n += 1
        if dsem is not None:
            dsem.count += 16
            o.dval = dsem.count
        writes = list(writes) + [b for b in reads if b.excl]
        reads = [b for b in reads if not b.excl]
        deps = {}
        raw = set()
        for d in extra:
            deps[d.id] = d
            raw.add(d.id)
        for b in reads:
            for d in b.w.values():
                deps[d.id] = d
                raw.add(d.id)
        for b in writes:
            for d in b.r.values():
                deps[d.id] = d
            for d in b.w.values():
                deps[d.id] = d
        o.deps = []
        for d in deps.values():
            if d is o:
                continue
            if d.dsem is None:
                same = d.eng == eng and dsem is None
                if (not same) or (d.id in raw and eng != "pe"):
                    d.signaled = True
                    o.deps.append(d)
            else:
                if dsem is not None and d.dsem is dsem and d.id not in raw:
                    continue
                o.deps.append(d)
        key = eng if dsem is None else ("d", id(dsem))
        for b in reads:
            b.r[key] = o
        for b in writes:
            if b.r:
                b.r = {}
                b.w = {key: o}
            else:
                b.w[key] = o
        self.ops[eng].append(o)
        self.last[eng] = o
        return o

    def finalize(self):
        for e in ENGS:
            c = 0
            for o in self.ops[e]:
                if o.signaled and o.dsem is None:
                    c += 1
                    o.count = c

    def emit(self, eng, h, psem):
        waited = {}
        for o in self.ops[eng]:
            need = {}
            for d in o.deps:
                if d.dsem is not None:
                    k, sem, v = ("d", id(d.dsem)), d.dsem.sem, d.dval
                else:
                    k, sem, v = d.eng, psem[d.eng], d.count
                if k not in need or need[k][1] < v:
                    need[k] = (sem, v)
            for k, (sem, v) in need.items():
                if waited.get(k, 0) < v:
                    h.wait_ge(sem, v)
                    waited[k] = v
            ins = o.fn(h)
            if o.dsem is not None:
                ins.then_inc(o.dsem.sem, 16)
            elif o.signaled:
                ins.then_inc(psem[eng], 1)


def build_program():
    nc = bass.Bass("TRN2", target_bir_lowering=False)
    P = Prog()

    def din(name, shape, dt=F32):
        return nc.dram_tensor(name, list(shape), dt, kind="ExternalInput").ap()

    xT_d = din("xT", [NSEQ, D, S])
    x_d = din("x", [NSEQ, S, D])
    w_in_d = din("w_in", [D, 6656])
    wpr_d = din("w_proj_ret", [1024, 1024])
    wpa_d = din("w_proj_attn", [512, 1024])
    wout_d = din("w_out", [1024, 1024])
    wg_d = din("w_gate", [16, 1024, 512])
    wu_d = din("w_up", [16, 1024, 512])
    wd_d = din("w_down", [16, 512, 1024])
    dec_d = din("dec", [128, 8])
    gng_d = din("gng", [128, 8])
    ln1g_d = din("ln1g", [128, 1024])
    ln1b_d = din("ln1b", [128, 1024])
    ln2g_d = din("ln2g", [128, 1024])
    ln2b_d = din("ln2b", [128, 1024])
    rw_d = din("rw", [1024, 20])
    rb_d = din("rb", [128, 20])
    tb_d = din("tb", [128, 24 * 256])
    cos_d = din("cosT", [128, S])
    sin_d = din("sinT", [128, S])
    cst_d = din("cst", [128, 5 * 128 + 2])
    out_d = nc.dram_tensor("out", [NSEQ, S, D], F32, kind="ExternalOutput").ap()

    w_in_v = w_in_d.rearrange("(kc p) c -> p kc c", p=128)
    wpr_v = wpr_d.rearrange("(kc p) c -> p kc c", p=128)
    wpa_v = wpa_d.rearrange("(kc p) c -> p kc c", p=128)
    wout_v = wout_d.rearrange("(kc p) c -> p kc c", p=128)
    rw_v = rw_d.rearrange("(kc p) c -> p kc c", p=128)

    es = ExitStack()
    with es:
        ARENA_BYTES = 174 * 1024
        arena = es.enter_context(nc.sbuf_tensor("arena", [128, ARENA_BYTES], U8))[:, :]
        PB = [es.enter_context(nc.psum_tensor(f"pb{i}", [128, 512], F32))[:, :] for i in range(8)]
        PBB = [[Buf(excl=True)] for _ in range(8)]
        psem = {e: es.enter_context(nc.semaphore(f"prog_{e}")) for e in ENGS}
        dsems = []

        def new_dsem():
            s_ = DSem(es.enter_context(nc.semaphore(f"dma{len(dsems)}")))
            dsems.append(s_)
            return s_

        def view(off, shape, dt):
            n = 1
            for v_ in shape[1:]:
                n *= v_
            nb = n * DSIZE[dt]
            assert off + nb <= ARENA_BYTES, (off, nb)
            v = arena[:, off:off + nb].bitcast(dt)
            if len(shape) == 3:
                v = v.rearrange("p (a b) -> p a b", b=shape[2])
            elif len(shape) == 4:
                v = v.rearrange("p (a b c) -> p a b c", b=shape[2], c=shape[3])
            return v

        KB = 1024

        class Carver:
            def __init__(self, regions):
                self.regions = [list(r) for r in regions]

            def get(self, shape, dt):
                n = 1
                for v_ in shape[1:]:
                    n *= v_
                nb = (n * DSIZE[dt] + 31) // 32 * 32
                for r in self.regions:
                    if r[1] - r[0] >= nb:
                        off = r[0]
                        r[0] += nb
                        return view(off, shape, dt)
                raise RuntimeError(f"arena overflow {shape}")

        def mm(out, lhsT, rhs, start, stop, r, w):
            return P.add("pe", lambda e: e.matmul(out, lhsT=lhsT, rhs=rhs, start=start, stop=stop,
                                                  skip_group_check=True), r, w)

        def tr(out, in_, ident, r, w):
            return P.add("pe", lambda e: e.transpose(out, in_, ident), r, w)

        def act(out, in_, func, r, w, bias=None, scale=None, accum=None):
            kw = {}
            if bias is not None:
                kw["bias"] = bias
            if scale is not None:
                kw["scale"] = scale
            if accum is not None:
                kw["accum_out"] = accum
            return P.add("act", lambda e: e.activation(out=out, in_=in_, func=func, **kw), r, w)

        def tt(eng, out, a, b, op, r, w):
            return P.add(eng, lambda e: e.tensor_tensor(out=out, in0=a, in1=b, op=op), r, w)

        def ts(eng, out, a, s1, s2, op0, op1, r, w):
            if op1 is None:
                return P.add(eng, lambda e: e.tensor_scalar(out=out, in0=a, scalar1=s1, scalar2=None, op0=op0), r, w)
            return P.add(eng, lambda e: e.tensor_scalar(out=out, in0=a, scalar1=s1, scalar2=s2, op0=op0, op1=op1), r, w)

        def stt(out, a, s_, b, op0, op1, r, w):
            return P.add("dve", lambda e: e.scalar_tensor_tensor(out=out, in0=a, scalar=s_, in1=b, op0=op0, op1=op1), r, w)

        def cp(eng, out, in_, r, w):
            if eng == "act":
                return P.add("act", lambda e: e.activation(out=out, in_=in_, func=AF.Copy), r, w)
            return P.add(eng, lambda e: e.tensor_copy(out=out, in_=in_), r, w)

        def memset(eng, ap, val, w):
            return P.add(eng, lambda e: e.memset(ap, val), (), w)

        def dma(eng, out, in_, r, w, dsem):
            o = P.add(eng, lambda e: e.dma_start(out=out, in_=in_), r, w, dsem=dsem)
            pending_dma.append(o)
            return o

        pending_dma = []
        dbg_sem = [None]

        def dump(name, ap, shape, dt, reads):
            if not DBG:
                return
            if dbg_sem[0] is None:
                dbg_sem[0] = new_dsem()
            t = nc.dram_tensor("dbg_" + name, list(shape), dt, kind="ExternalOutput").ap()
            dma("sync", t, ap, reads, (), dbg_sem[0])

        def barrier():
            b = Buf()
            for o in pending_dma:
                key = ("d", id(o.dsem))
                if key not in b.w or b.w[key].dval < o.dval:
                    b.w[key] = o
            del pending_dma[:]
            for e in ENGS:
                ex = [P.last[e]] if (e in P.last and e != "pe" and P.last[e].dsem is None) else []
                P.add(e, lambda h: h.nop(), (), [b], extra=ex)
            for e in ENGS:
                P.add(e, lambda h: h.nop(), [b], ())

        pc = Carver([(0, 10 * KB)])
        LG = pc.get([128, 8], F32)
        CD = pc.get([128, 8], F32)
        ZF = pc.get([128, 4], F32)
        ZB = pc.get([128, 4], F32)
        CONS = pc.get([128, 8], F32)
        DT = pc.get([128, 4, 128], F32)
        XIF = pc.get([128, 4, 128], F32)
        XIB = pc.get([128, 4, 128], F32)
        IDB = pc.get([128, 128], BF16)
        IDF = pc.get([128, 128], F32)
        RW = pc.get([128, 8, 20], F32)
        RB = pc.get([128, 20], F32)
        CW = pc.get([128, 16, 16], F32)
        ZERO = pc.get([128, 512], BF16)
        bPers = Buf()
        bCW = Buf()

        XT = view(10 * KB, [128, 8, S], BF16)
        OGT = view(42 * KB, [128, 8, S], BF16)
        ATT = view(74 * KB, [128, 4, S], BF16)
        MRG = view(90 * KB, [128, 8, S], BF16)
        X1 = view(10 * KB, [128, 16, 1024], F32)
        X1T = view(122 * KB, [128, 8, S], BF16)
        bXT, bOGT, bATT, bMRG, bX1T = Buf(), Buf(), Buf(), Buf(), Buf()
        bX1 = [Buf() for _ in range(16)]

        def load_xt(s_, sems_, extra_w=(), tgs=(0, 1, 2, 3)):
            xT_v = xT_d[s_].rearrange("(kc p) t -> p kc t", p=128)
            ops_ = []
            for tg in tgs:
                ops_.append(dma("pool", XT[:, :, tg * 512:(tg + 1) * 512], xT_v[:, :, tg * 512:(tg + 1) * 512], (),
                                [bXTg[tg]] + (list(extra_w) if tg == 0 else []), sems_[tg]))
            return ops_

        bXTg = [Buf() for _ in range(4)]
        sem_xtg = [new_dsem() for _ in range(4)]
        for o_ in load_xt(0, sem_xtg, tgs=(0,)):
            pending_dma.remove(o_)
        sc = Carver([(90 * KB, 170 * KB)])
        CST = sc.get([128, 642], F32)
        DEC = sc.get([128, 8], F32)
        TA = sc.get([128, 128], F32)
        TBm = sc.get([128, 128], F32)
        T8 = sc.get([128, 8], F32)
        sem_set = new_dsem()
        bset = Buf()
        dma("sync", CST, cst_d, (), [bset], sem_set)
        dma("sync", DEC, dec_d, (), [bset], sem_set)
        sem_set2 = new_dsem()
        dma("sync", RW, rw_v, (), [bPers], sem_set2)
        dma("sync", RB, rb_d, (), [bPers], sem_set2)
        P1 = CST[:, 0:128]
        P2 = CST[:, 128:256]
        EYE = CST[:, 256:384]
        POSF = CST[:, 384:512]
        POSB = CST[:, 512:640]
        PCOL = CST[:, 640:642]
        memset("pool", CONS[:, 0:1], 1.0, [bPers])
        memset("pool", CONS[:, 1:2], LNCK, [bPers])
        memset("pool", CONS[:, 2:3], EPS, [bPers])
        memset("pool", CONS[:, 3:4], EPS / (ALPHA * ALPHA), [bPers])
        memset("pool", CONS[:, 4:5], -0.5, [bPers])
        memset("pool", ZERO, 0.0, [bPers])
        cp("dve", IDB, EYE, [bset], [bPers])
        cp("dve", IDF, EYE, [bset], [bPers])
        act(T8, DEC, AF.Exp, [bset], [bset], scale=-1.0)
        act(T8, T8, AF.Ln, [bset, bPers], [bset], bias=CONS[:, 0:1])
        ts("dve", LG, T8, -1.0, None, ALU.mult, None, [bset], [bPers])
        act(CD, LG, AF.Exp, [bPers], [bPers], scale=128.0)
        for h in range(4):
            ts("dve", TA, P1, LG[:, h:h + 1], None, ALU.mult, None, [bset, bPers], [bset])
            stt(TBm, P2, LG[:, 4 + h:5 + h], TA, ALU.mult, ALU.add, [bset, bPers], [bset])
            act(DT[:, h, :], TBm, AF.Exp, [bset, bPers], [bPers], bias=CONS[:, 1:2])
            stt(DT[:, h, :], EYE, CK, DT[:, h, :], ALU.mult, ALU.add, [bset, bPers], [bPers])
            act(XIF[:, h, :], POSF, AF.Exp, [bset, bPers], [bPers], scale=LG[:, h:h + 1])
            act(XIB[:, h, :], POSB, AF.Exp, [bset, bPers], [bPers], scale=LG[:, 4 + h:5 + h])
            act(ZF[:, h:h + 1], PCOL[:, 0:1], AF.Exp, [bset, bPers], [bPers], scale=LG[:, h:h + 1], bias=CONS[:, 1:2])
            act(ZB[:, h:h + 1], PCOL[:, 1:2], AF.Exp, [bset, bPers], [bPers], scale=LG[:, 4 + h:5 + h], bias=CONS[:, 1:2])
        dump("lg", LG, [128, 8], F32, [bPers])
        dump("cd", CD, [128, 8], F32, [bPers])
        dump("zf", ZF, [128, 4], F32, [bPers])
        dump("dt", DT, [128, 4, 128], F32, [bPers])
        dump("xif", XIF, [128, 4, 128], F32, [bPers])
        barrier()

        out_sems = [new_dsem() for _ in range(16)]
        out_ops = []
        sem_x = [new_dsem() for _ in range(16)]

        def stat_pool(carver, n):
            return [dict(st=carver.get([128, 12], F32), mv=carver.get([128, 2], F32), ve=carver.get([128, 1], F32),
                         rs=carver.get([128, 1], F32), nm=carver.get([128, 1], F32), b=Buf(), b2=Buf()) for _ in range(n)]

        def norm_stats(src_aps, src_bufs, sp, eps=EPS, need_nm=False, use_pool=False):
            b = sp["b"]
            for i, a in enumerate(src_aps):
                P.add("dve", (lambda a_, o_: (lambda e: e.bn_stats(out=o_, in_=a_)))(a, sp["st"][:, 6 * i:6 * i + 6]),
                      src_bufs, [b])
            nst = len(src_aps) * 6
            P.add("dve", lambda e: e.bn_aggr(out=sp["mv"], in_=sp["st"][:, 0:nst]), [b], [b])
            b2 = sp["b2"]
            if use_pool:
                ts("dve", sp["ve"], sp["mv"][:, 1:2], eps, None, ALU.add, None, [b], [b2])
                tt("pool", sp["rs"], sp["ve"], CONS[:, 4:5], ALU.pow, [b2, bPers], [b2])
            else:
                act(sp["ve"], sp["mv"][:, 1:2], AF.Ln, [b, bPers], [b2], bias=(CONS[:, 2:3] if eps == EPS else CONS[:, 3:4]))
                act(sp["rs"], sp["ve"], AF.Exp, [b2], [b2], scale=-0.5)
            if need_nm:
                ts("dve", sp["nm"], sp["mv"][:, 0:1], sp["rs"], -1.0, ALU.mult, ALU.mult, [b, b2], [b2])

        def ln_affine(tile_ap, tb_, sp, G, Bv, bG, eps=EPS, use_pool=False):
            norm_stats([tile_ap[:, 0:512], tile_ap[:, 512:1024]], [tb_], sp, eps=eps, use_pool=use_pool)
            stt(tile_ap, tile_ap, sp["mv"][:, 0:1], G, ALU.subtract, ALU.mult, [tb_, sp["b"], bG], [tb_])
            stt(tile_ap, tile_ap, sp["rs"], Bv, ALU.mult, ALU.add, [tb_, sp["b2"], bG], [tb_])

        seq_pool = []
        seq_idx = [0]

        def seq_dsem():
            i_ = seq_idx[0]
            seq_idx[0] += 1
            if i_ == len(seq_pool):
                seq_pool.append(new_dsem())
            return seq_pool[i_]

        for s in range(NSEQ):
            seq_idx[0] = 0
            rc = Carver([(74 * KB, 174 * KB)])
            COS = rc.get([128, S], F32)
            SINX = rc.get([128, S], F32)
            GNG = rc.get([128, 8], F32)
            WH = rc.get([128, 8, 768], BF16)
            QROT = rc.get([128, S], BF16)
            KROT = rc.get([128, S], BF16)
            QXF = rc.get([128, S], BF16)
            QXB = rc.get([128, S], BF16)
            KZF = rc.get([128, 16, 128], BF16)
            KZB = rc.get([128, 16, 128], BF16)
            V = rc.get([128, 16, 256], BF16)
            GG = rc.get([128, 16, 256], BF16)
            SFALL = rc.get([128, 16, 256], BF16)
            SBALL = rc.get([128, 16, 256], BF16)
            SF32 = rc.get([128, 256], F32)
            SB32 = rc.get([128, 256], F32)
            T1 = [rc.get([128, 512], F32) for _ in range(2)]
            T2 = [rc.get([128, 512], F32) for _ in range(2)]
            bT = [Buf(), Buf()]
            STM = [rc.get([128, 128], BF16) for _ in range(3)]
            bSTM = [Buf() for _ in range(3)]
            ON = [rc.get([128, 256], F32) for _ in range(2)]
            OG = [rc.get([128, 256], BF16) for _ in range(4)]
            bON = [Buf(), Buf()]
            bOG = [Buf() for _ in range(4)]
            SP = stat_pool(rc, 4)
            bRC, bWH, bQ, bK, bQX, bKZ, bV, bGG, bSA, bS32, bS32B = (Buf() for _ in range(11))

            sem_r = seq_dsem()
            sem_xt = seq_dsem()
            sem_wh = seq_dsem()
            dma("sync", COS, cos_d, (), [bRC], sem_r)
            dma("sync", SINX, sin_d, (), [bRC], sem_r)
            dma("sync", GNG, gng_d, (), [bRC], sem_r)

            hs = [0]

            def halfslot(banks):
                bk = banks[hs[0] % len(banks)]
                hs[0] += 1
                return PB[bk][:, 0:256], PBB[bk]

            fb = [0]

            def fullbank(banks):
                bk = banks[fb[0] % len(banks)]
                fb[0] += 1
                return PB[bk], PBB[bk]

            rot = [0]

            def load_wh(h):
                for (c0, n_, d0) in ((h * 128, 128, 0), (512 + h * 128, 128, 128), (1024 + h * 256, 256, 256),
                                     (2048 + h * 256, 256, 512)):
                    dma("pool", WH[:, :, d0:d0 + n_], w_in_v[:, :, c0:c0 + n_], (), [bWH], sem_wh)

            load_wh(0)
            if s == 0:
                load_xt(0, sem_xtg, tgs=(1, 2, 3))
            for h in range(4):
                for which in range(2):
                    dst = QROT if which == 0 else KROT
                    bdst = bQ if which == 0 else bK
                    for tg in range(4):
                        bank, bb = fullbank([0, 1])
                        for kc in range(8):
                            mm(bank, WH[:, kc, which * 128:(which + 1) * 128], XT[:, kc, tg * 512:(tg + 1) * 512],
                               kc == 0, kc == 7, [bWH, bXTg[tg]], bb)
                        k_ = rot[0] % 2
                        rot[0] += 1
                        tsl = slice(tg * 512, (tg + 1) * 512)
                        tt("dve", T1[k_], bank, COS[:, tsl], ALU.mult, bb + [bRC], [bT[k_]])
                        tt("dve", T2[k_][0:64, :], bank[64:128, :], SINX[64:128, tsl], ALU.mult, bb + [bRC], [bT[k_]])
                        tt("dve", T2[k_][64:128, :], bank[0:64, :], SINX[0:64, tsl], ALU.mult, bb + [bRC], [bT[k_]])
                        tt("dve", dst[:, tsl], T1[k_], T2[k_], ALU.add, [bT[k_]], [bdst])
                q3 = QROT.rearrange("p (n c) -> p n c", c=128)
                tt("dve", QXF.rearrange("p (n c) -> p n c", c=128), q3,
                   XIF[:, h, :].unsqueeze(1).broadcast_to([128, 16, 128]), ALU.mult, [bQ, bPers], [bQX])
                tt("dve", QXB.rearrange("p (n c) -> p n c", c=128), q3,
                   XIB[:, h, :].unsqueeze(1).broadcast_to([128, 16, 128]), ALU.mult, [bQ, bPers], [bQX])
                memset("pool", SF32, 0.0, [bS32])
                memset("pool", SB32, 0.0, [bS32B])
                memset("pool", SFALL[:, 0, :], 0.0, [bSA])
                memset("pool", SBALL[:, 15, :], 0.0, [bSA])

                def ktr(n):
                    slot, sb = halfslot([2, 3])
                    sbf = slot.bitcast(BF16)
                    tr(sbf[:, 0:128], KROT[:, n * 128:(n + 1) * 128], IDB, [bK, bPers], sb)
                    act(KZF[:, n, :], sbf[:, 0:128], AF.Identity, sb + [bPers], [bKZ], scale=ZF[:, h:h + 1])
                    ts("dve", KZB[:, n, :], sbf[:, 0:128], ZB[:, h:h + 1], None, ALU.mult, None, sb + [bPers], [bKZ])

                def vg(n):
                    bank, bb = fullbank([0, 1])
                    for kc in range(8):
                        mm(bank, XT[:, kc, n * 128:(n + 1) * 128], WH[:, kc, 256:768], kc == 0, kc == 7, [bWH, bXTg[n // 4]], bb)
                    cp("dve", V[:, n, :], bank[:, 0:256], bb, [bV])
                    act(GG[:, n, :], bank[:, 256:512], AF.Silu, bb, [bGG])

                def fstep(n):
                    slot, sb = halfslot([2, 3])
                    mm(slot, KZF[:, n, :], V[:, n, :], True, True, [bKZ, bV], sb)
                    stt(SF32, SF32, CD[:, h:h + 1], slot, ALU.mult, ALU.add, sb + [bS32, bPers], [bS32])
                    cp("act", SFALL[:, n + 1, :], SF32, [bS32], [bSA])

                def bstep(m):
                    slot, sb = halfslot([2, 3])
                    mm(slot, KZB[:, m, :], V[:, m, :], True, True, [bKZ, bV], sb)
                    stt(SB32, SB32, CD[:, 4 + h:5 + h], slot, ALU.mult, ALU.add, sb + [bS32B, bPers], [bS32B])
                    cp("act", SBALL[:, m - 1, :], SB32, [bS32B], [bSA])

                def P_st2(j):
                    st_ = j % 2
                    kb = 2 + st_
                    kbf = PB[kb][:, 0:256].bitcast(BF16)
                    for q2, n in enumerate((j, 15 - j)):
                        tr(kbf[:, q2 * 128:(q2 + 1) * 128], KROT[:, n * 128:(n + 1) * 128], IDB, [bK, bPers], PBB[kb])
                    for q2, n in enumerate((j, 15 - j)):
                        vb = (0, 1, 4, 5)[2 * st_ + q2]
                        for kc in range(8):
                            mm(PB[vb], XT[:, kc, n * 128:(n + 1) * 128], WH[:, kc, 256:768], kc == 0, kc == 7,
                               [bWH, bXTg[n // 4]], PBB[vb])

                def E_st2(j):
                    st_ = j % 2
                    kb = 2 + st_
                    kbf = PB[kb][:, 0:256].bitcast(BF16)
                    for q2, n in enumerate((j, 15 - j)):
                        act(KZF[:, n, :], kbf[:, q2 * 128:(q2 + 1) * 128], AF.Identity, PBB[kb] + [bPers], [bKZ], scale=ZF[:, h:h + 1])
                        ts("dve", KZB[:, n, :], kbf[:, q2 * 128:(q2 + 1) * 128], ZB[:, h:h + 1], None, ALU.mult, None,
                           PBB[kb] + [bPers], [bKZ])
                    for q2, n in enumerate((j, 15 - j)):
                        vb = (0, 1, 4, 5)[2 * st_ + q2]
                        cp("dve", V[:, n, :], PB[vb][:, 0:256], PBB[vb], [bV])
                        act(GG[:, n, :], PB[vb][:, 256:512], AF.Silu, PBB[vb], [bGG])

                def SP_st2(j):
                    mm(PB[6][:, 0:256], KZF[:, j, :], V[:, j, :], True, True, [bKZ, bV], PBB[6])
                    mm(PB[7][:, 0:256], KZB[:, 15 - j, :], V[:, 15 - j, :], True, True, [bKZ, bV], PBB[7])

                def SE_st2(j):
                    stt(SF32, SF32, CD[:, h:h + 1], PB[6][:, 0:256], ALU.mult, ALU.add, PBB[6] + [bS32, bPers], [bS32])
                    cp("act", SFALL[:, j + 1, :], SF32, [bS32], [bSA])
                    stt(SB32, SB32, CD[:, 4 + h:5 + h], PB[7][:, 0:256], ALU.mult, ALU.add, PBB[7] + [bS32B, bPers], [bS32B])
                    cp("act", SBALL[:, 14 - j, :], SB32, [bS32B], [bSA])

                for i in range(8 + 2):
                    if 0 <= i - 1 < 8:
                        E_st2(i - 1)
                    if 0 <= i - 2 < 8:
                        SE_st2(i - 2)
                    if i < 8:
                        P_st2(i)
                    if 0 <= i - 1 < 8:
                        SP_st2(i - 1)
                if h < 3:
                    load_wh(h + 1)

                oorder = [7, 8]
                for k in range(1, 8):
                    oorder += [8 + k, 7 - k]

                slots = {}

                def P1(p):
                    n = oorder[p]
                    sl = slice(n * 128, (n + 1) * 128)
                    slot, sb = PB[p % 2][:, 0:256], PBB[p % 2]
                    slots[("s", p)] = (slot, sb)
                    mm(slot[:, 0:128], KROT[:, sl], QROT[:, sl], True, True, [bK, bQ], sb)

                def D1(p):
                    k3 = p % 3
                    slot, sb = slots.pop(("s", p))
                    tt("dve", STM[k3], slot[:, 0:128], DT[:, h, :], ALU.mult, sb + [bPers], [bSTM[k3]])

                def P2(p):
                    n = oorder[p]
                    sl = slice(n * 128, (n + 1) * 128)
                    k3 = p % 3
                    oslot, ob = PB[4 + p % 2][:, 0:256], PBB[4 + p % 2]
                    slots[("o", p)] = (oslot, ob)
                    mm(oslot, STM[k3], V[:, n, :], True, False, [bSTM[k3], bV], ob)
                    mm(oslot, QXF[:, sl], SFALL[:, n, :], False, False, [bQX, bSA], ob)
                    mm(oslot, QXB[:, sl], SBALL[:, n, :], False, True, [bQX, bSA], ob)

                def D2(p):
                    n = oorder[p]
                    k2 = p % 2
                    k4 = p % 4
                    oslot, ob = slots.pop(("o", p))
                    sp = SP[p % 4]
                    norm_stats([oslot], ob, sp)
                    stt(ON[k2], oslot, sp["mv"][:, 0:1], GG[:, n, :], ALU.subtract, ALU.mult, ob + [sp["b"], bGG], [bON[k2]])
                    act(OG[k4], ON[k2], AF.Identity, [bON[k2], sp["b2"]], [bOG[k4]], scale=sp["rs"])

                def P3(p):
                    k4 = p % 4
                    tslot, tb_ = PB[6 + p % 2][:, 0:256], PBB[6 + p % 2]
                    tbf = tslot.bitcast(BF16)
                    slots[("t", p)] = (tbf, tb_)
                    tr(tbf[:, 0:128], OG[k4][:, 0:128], IDB, [bOG[k4], bPers], tb_)
                    tr(tbf[:, 128:256], OG[k4][:, 128:256], IDB, [bOG[k4], bPers], tb_)

                def C3(p):
                    n = oorder[p]
                    sl = slice(n * 128, (n + 1) * 128)
                    tbf, tb_ = slots.pop(("t", p))
                    for j2 in range(2):
                        act(OGT[:, 2 * h + j2, sl], tbf[:, j2 * 128:(j2 + 1) * 128], AF.Identity, tb_ + [bRC], [bOGT],
                            scale=GNG[:, 2 * h + j2:2 * h + j2 + 1])

                for i in range(16 + 6):
                    if i < 16:
                        P1(i)
                    if 0 <= i - 1 < 16:
                        q_ = i - 1
                        D1(q_)
                        if q_ >= 2 and q_ % 2 == 0:
                            k = q_ // 2
                            fstep(7 + k)
                            bstep(8 - k)
                    if 0 <= i - 2 < 16:
                        P2(i - 2)
                    if 0 <= i - 3 < 16:
                        D2(i - 3)
                    if 0 <= i - 5 < 16:
                        P3(i - 5)
                    if 0 <= i - 6 < 16:
                        C3(i - 6)
                if s == 0:
                    dump(f"qrot{h}", QROT, [128, S], BF16, [bQ])
                    dump(f"krot{h}", KROT, [128, S], BF16, [bK])
                    dump(f"v{h}", V, [128, 16, 256], BF16, [bV])
                    dump(f"sfall{h}", SFALL, [128, 16, 256], BF16, [bSA])
                    dump(f"sball{h}", SBALL, [128, 16, 256], BF16, [bSA])
                    dump(f"qxf{h}", QXF, [128, S], BF16, [bQX])
                    dump(f"qxb{h}", QXB, [128, S], BF16, [bQX])
            if s == 0:
                dump("ogt", OGT, [128, 8, S], BF16, [bOGT])
                dump("xt", XT, [128, 8, S], BF16, bXTg)
            barrier()

            ac = Carver([(90 * KB, 174 * KB)])
            TB = ac.get([128, 24, 256], BF16)
            WA = ac.get([128, 8, 1536], BF16)
            QAM = [ac.get([128, S], BF16) for _ in range(2)]
            KA = ac.get([128, S], BF16)
            VT = ac.get([128, S], BF16)
            bVT = Buf()
            VAUG = ac.get([128, 96, 128], BF16)
            NPT = 4
            TMP = [ac.get([128, 256], F32) for _ in range(NPT)]
            PT = [ac.get([128, 256], BF16) for _ in range(NPT)]
            bTMP = [Buf() for _ in range(NPT)]
            bPT = [Buf() for _ in range(NPT)]
            ZR = [ac.get([128, 512], F32)] * 2
            bZR = [Buf()] * 2
            bTB, bWA, bQA, bKA, bVA = (Buf() for _ in range(5))
            sem_tb = seq_dsem()
            sem_wa = seq_dsem()
            bWA3 = [Buf(), Buf(), Buf()]
            sem_wa3 = [sem_wa, seq_dsem(), seq_dsem()]
            for j3 in range(3):
                dma("pool", WA[:, :, j3 * 512:(j3 + 1) * 512], w_in_v[:, :, 3072 + j3 * 512:3072 + (j3 + 1) * 512], (),
                    [bWA3[j3]], sem_wa3[j3])
            dma("pool", TB, tb_d.rearrange("p (a b) -> p a b", b=256), (), [bTB], sem_tb)
            memset("pool", VAUG[:, :, 64:128], 1.0, [bVA])
            memset("pool", QAM[0][64:128, :], 0.0, [bQA])
            memset("pool", QAM[1][0:64, :], 0.0, [bQA])
            for hp in range(4):
                for which in range(2):
                    for tg in range(4):
                        bank, bb = fullbank([4, 5])
                        c0 = which * 512 + hp * 128
                        for kc in range(8):
                            mm(bank, WA[:, kc, c0:c0 + 128], XT[:, kc, tg * 512:(tg + 1) * 512],
                               kc == 0, kc == 7, [bWA3[which], bXTg[tg]], bb)
                        tsl = slice(tg * 512, (tg + 1) * 512)
                        if which == 0:
                            act(QAM[0][0:64, tsl], bank[0:64, :], AF.Copy, bb, [bQA], scale=0.125)
                            act(QAM[1][64:128, tsl], bank[64:128, :], AF.Copy, bb, [bQA], scale=0.125)
                        else:
                            cp("act", KA[:, tsl], bank, bb, [bKA])
                for tg in range(4):
                    bank, bb = fullbank([4, 5])
                    for kc in range(8):
                        mm(bank, WA[:, kc, 1024 + hp * 128:1024 + (hp + 1) * 128], XT[:, kc, tg * 512:(tg + 1) * 512],
                           kc == 0, kc == 7, [bWA3[2], bXTg[tg]], bb)
                    cp("act", VT[:, tg * 512:(tg + 1) * 512], bank, bb, [bVT])
                for pi, d in enumerate(PATTERNS):
                    nsub = S // d
                    nb = nsub // 128
                    for r in range(d):
                        for b in range(nb):
                            blk = r * nb + b
                            slot, sb = halfslot([6, 7])
                            sbf = slot.bitcast(BF16)
                            vin = VT[:, :].rearrange("p (m r) -> p r m", r=d)[:, r, b * 128:(b + 1) * 128]
                            tr(sbf[:, 0:128], vin, IDB, [bVT, bPers], sb)
                            i0 = (pi * 16 + blk) * 2
                            cp("dve", VAUG[:, i0:i0 + 2, 0:64], sbf[:, 0:128].rearrange("p (a b) -> p a b", b=64), sb, [bVA])
                items = [(s2, pi, d, r, b) for s2 in range(2) for pi, d in enumerate(PATTERNS) for r in range(d)
                         for b in range(S // d // 128)]
                nper = len(items) // 2

                ast = {}

                def A1(i):
                    s2, pi, d, r, b = items[i]
                    nsub = S // d
                    KAv = KA[:, :].rearrange("p (m r) -> p r m", r=d)
                    QAv = QAM[s2][:, :].rearrange("p (m r) -> p r m", r=d)
                    a = 128 * b
                    q_lo = max(a - 64, 0)
                    q_hi = min(a + 192, nsub)
                    N = q_hi - q_lo
                    bk = 4 + i % 4
                    slot, sb = PB[bk][:, 0:256], PBB[bk]
                    ast[i] = (slot, sb)
                    mm(slot[:, 0:N], KAv[:, r, a:a + 128], QAv[:, r, q_lo:q_hi], True, True, [bKA, bQA], sb)

                def A1b(i):
                    s2, pi, d, r, b = items[i]
                    h = 2 * hp + s2
                    nsub = S // d
                    a = 128 * b
                    q_lo = max(a - 64, 0)
                    q_hi = min(a + 192, nsub)
                    N = q_hi - q_lo
                    j_lo = q_lo - (a - 64)
                    slot, sb = ast.pop(i)
                    k_ = i % NPT
                    tt("dve", TMP[k_][:, 0:N], slot[:, 0:N], TB[:, pi * 8 + h, j_lo:j_lo + N], ALU.add,
                       sb + [bTB], [bTMP[k_]])

                def A1c(i):
                    s2, pi, d, r, b = items[i]
                    nsub = S // d
                    a = 128 * b
                    N = min(a + 192, nsub) - max(a - 64, 0)
                    k_ = i % NPT
                    act(PT[k_][:, 0:N], TMP[k_][:, 0:N], AF.Exp, [bTMP[k_]], [bPT[k_]])

                def A2(i):
                    s2, pi, d, r, b = items[i]
                    prt = slice(64 * s2, 64 * s2 + 64)
                    nsub = S // d
                    nb = nsub // 128
                    blk = r * nb + b
                    a = 128 * b
                    q_lo = max(a - 64, 0)
                    q_hi = min(a + 192, nsub)
                    k_ = i % NPT
                    if i % nper == 0:
                        for k in range(4):
                            mm(PB[k], ZERO[:, 0:128], ZERO[:, 0:512], True, True, [bPers], PBB[k])
                    i0 = (pi * 16 + blk) * 2 + s2
                    per = 512 // d
                    for k in range(4):
                        m0 = max(q_lo, k * per)
                        m1 = min(q_hi, (k + 1) * per)
                        if m1 <= m0:
                            continue
                        ov = PB[k][:, :].rearrange("p (m r) -> p r m", r=d)[:, r, m0 - k * per:m1 - k * per]
                        mm(ov, VAUG[:, i0, :], PT[k_][:, m0 - q_lo:m1 - q_lo], False, False,
                           [bVA, bPT[k_]], PBB[k])
                    if i % nper == nper - 1:
                        for k in range(4):
                            z_ = k % 2
                            act(ZR[z_][64:128, :], PB[k][64:128, :], AF.Ln, PBB[k], [bZR[z_]])
                            act(ZR[z_][0:64, :], ZR[z_][64:128, :], AF.Exp, [bZR[z_]], [bZR[z_]], scale=-1.0)
                            tt("dve", ATT[prt, hp, k * 512:(k + 1) * 512], PB[k][0:64, :], ZR[z_][0:64, :], ALU.mult,
                               PBB[k] + [bZR[z_]], [bATT])

                nit = len(items)
                for i in range(nit + 4):
                    if i < nit:
                        A1(i)
                    if 0 <= i - 1 < nit:
                        A1b(i - 1)
                    if 0 <= i - 2 < nit:
                        A1c(i - 2)
                    if 0 <= i - 4 < nit:
                        A2(i - 4)
            if s == 0:
                dump("att", ATT, [128, 4, S], BF16, [bATT])
            barrier()

            mc = Carver([(122 * KB, 174 * KB)])
            WM = [mc.get([128, 28, 256], BF16) for _ in range(2)]
            bWM = [Buf(), Buf()]
            sem_wm = [seq_dsem(), seq_dsem()]
            SR = [mc.get([128, 512], F32) for _ in range(2)]
            SA_ = [mc.get([128, 512], F32) for _ in range(2)]
            M1 = [mc.get([128, 512], F32) for _ in range(2)]
            M2 = [mc.get([128, 512], F32) for _ in range(2)]
            bSR = [Buf(), Buf()]
            bSA2 = [Buf(), Buf()]
            bM1 = [Buf(), Buf()]
            bM2 = [Buf(), Buf()]
            it = 0
            def load_wm(c2):
                w_ = c2 % 2
                csl = slice(c2 * 256, (c2 + 1) * 256)
                dma("pool", WM[w_][:, 0:8, :], wpr_v[:, :, csl], (), [bWM[w_]], sem_wm[w_])
                dma("pool", WM[w_][:, 8:16, :], w_in_v[:, :, 4608 + c2 * 256:4608 + (c2 + 1) * 256], (), [bWM[w_]], sem_wm[w_])
                dma("pool", WM[w_][:, 16:20, :], wpa_v[:, :, csl], (), [bWM[w_]], sem_wm[w_])
                dma("pool", WM[w_][:, 20:28, :], w_in_v[:, :, 5632 + c2 * 256:5632 + (c2 + 1) * 256], (), [bWM[w_]], sem_wm[w_])

            load_wm(0)
            load_wm(1)
            for dc in range(8):
                c2 = dc // 2
                w_ = c2 % 2
                wsl = slice((dc % 2) * 128, (dc % 2) * 128 + 128)
                for tg in range(4):
                    tsl = slice(tg * 512, (tg + 1) * 512)
                    base = 4 * (it % 2)
                    k_ = it % 2
                    it += 1
                    bA, bB, bC, bD = base, base + 1, base + 2, base + 3
                    for kc in range(8):
                        mm(PB[bA], WM[w_][:, kc, wsl], OGT[:, kc, tsl], kc == 0, kc == 7, [bWM[w_], bOGT], PBB[bA])
                    for kc in range(8):
                        mm(PB[bB], WM[w_][:, 8 + kc, wsl], XT[:, kc, tsl], kc == 0, kc == 7, [bWM[w_], bXTg[tg]], PBB[bB])
                    for kc in range(4):
                        mm(PB[bC], WM[w_][:, 16 + kc, wsl], ATT[:, kc, tsl], kc == 0, kc == 3, [bWM[w_], bATT], PBB[bC])
                    for kc in range(8):
                        mm(PB[bD], WM[w_][:, 20 + kc, wsl], XT[:, kc, tsl], kc == 0, kc == 7, [bWM[w_], bXTg[tg]], PBB[bD])
                    act(SR[k_], PB[bB], AF.Sigmoid, PBB[bB], [bSR[k_]])
                    act(SA_[k_], PB[bD], AF.Sigmoid, PBB[bD], [bSA2[k_]])
                    tt("dve", M1[k_], SR[k_], PB[bA], ALU.mult, PBB[bA] + [bSR[k_]], [bM1[k_]])
                    tt("dve", M2[k_], SA_[k_], PB[bC], ALU.mult, PBB[bC] + [bSA2[k_]], [bM2[k_]])
                    tt("dve", MRG[:, dc, tsl], M1[k_], M2[k_], ALU.add, [bM1[k_], bM2[k_]], [bMRG])
                if dc % 2 == 1 and c2 + 2 < 4:
                    load_wm(c2 + 2)
            if s == 0:
                dump("mrg", MRG, [128, 8, S], BF16, [bMRG])
            barrier()

            m2c = Carver([(74 * KB, 90 * KB), (154 * KB, 174 * KB)])
            WOUT = m2c.get([128, 8, 1024], BF16)
            LNG = m2c.get([128, 1024], F32)
            LNB = m2c.get([128, 1024], F32)
            X1TF = m2c.get([128, 8, 128], F32)
            SP2 = stat_pool(m2c, 2)
            LGA = m2c.get([128, 16, 20], F32)
            RQ = dict((nm, m2c.get([128, 16], F32)) for nm in ("gm", "gs", "gw", "m1", "m2", "dif", "ex", "w1", "w2", "a1", "a2"))
            for nm in ("goh", "gd", "pen"):
                RQ[nm] = m2c.get([128, 16, 4], F32)
            for nm in ("em", "oh1", "oh2"):
                RQ[nm] = m2c.get([128, 16, 16], F32)
            bLGA, bRTB = Buf(), Buf()
            bWO, bLN, bX1TF = Buf(), Buf(), Buf()
            sem_m2 = seq_dsem()
            sem_wo = seq_dsem()
            dma("pool", WOUT, wout_v, (), [bWO], sem_wo)
            dma("sync", LNG, ln1g_d, (), [bLN], sem_m2)
            dma("sync", LNB, ln1b_d, (), [bLN], sem_m2)

            def M2a(t):
                sl = slice(t * 128, (t + 1) * 128)
                xt_ = X1[:, t, :]
                dma("sync", xt_, x_d[s, t * 128:(t + 1) * 128, :], (), [bX1[t]], sem_x[t])
                base = 2 * (t % 2)
                for half in range(2):
                    bk = base + half
                    for dc in range(8):
                        mm(PB[bk], MRG[:, dc, sl], WOUT[:, dc, half * 512:(half + 1) * 512], dc == 0, dc == 7,
                           [bMRG, bWO], PBB[bk])
                    hsl = slice(half * 512, (half + 1) * 512)
                    stt(xt_[:, hsl], xt_[:, hsl], ALPHA, PB[bk], ALU.mult, ALU.add, PBB[bk] + [bX1[t]], [bX1[t]])
                ln_affine(xt_, bX1[t], SP2[t % 2], LNG, LNB, bLN)

            def M2b(t):
                sl = slice(t * 128, (t + 1) * 128)
                xt_ = X1[:, t, :]
                for g in range(2):
                    bk = 4 + g
                    for q in range(4):
                        dc = g * 4 + q
                        tr(PB[bk][:, q * 128:(q + 1) * 128], xt_[:, dc * 128:(dc + 1) * 128], IDF, [bX1[t], bPers], PBB[bk])
                    pv = PB[bk][:, :].rearrange("p (a b) -> p a b", b=128)
                    cp("act", X1T[:, g * 4:g * 4 + 4, sl], pv, PBB[bk], [bX1T])
                    cp("act", X1TF[:, g * 4:g * 4 + 4, :], pv, PBB[bk], [bX1TF])

            def M2c(t):
                for dc in range(8):
                    mm(PB[6][:, 0:20], X1TF[:, dc, :], RW[:, dc, :], dc == 0, dc == 7, [bX1TF, bPers], PBB[6])
                tt("dve", LGA[:, t, :], PB[6][:, 0:20], RB, ALU.add, PBB[6] + [bPers], [bLGA])

            for i in range(18):
                if i >= 2:
                    M2b(i - 2)
                if i < 16:
                    M2a(i)
                if i >= 2:
                    M2c(i - 2)
            rb_ = [bRTB]

            def bc(y2, k):
                return y2.unsqueeze(2).broadcast_to([128, 16, k])

            def red(out, in_, op):
                return P.add("dve", lambda e: e.tensor_reduce(out=out, in_=in_, axis=AX.X, op=op), [bLGA] + rb_, rb_)

            def rcp(out, in_):
                return P.add("dve", lambda e: e.reciprocal(out=out, in_=in_), rb_, rb_)

            Gv = LGA[:, :, 0:4]
            Ev = LGA[:, :, 4:20]
            red(RQ["gm"], Gv, ALU.max)
            tt("dve", RQ["goh"], Gv, bc(RQ["gm"], 4), ALU.is_ge, [bLGA] + rb_, rb_)
            tt("dve", RQ["gd"], Gv, bc(RQ["gm"], 4), ALU.subtract, [bLGA] + rb_, rb_)
            act(RQ["gd"], RQ["gd"], AF.Exp, rb_, rb_)
            red(RQ["gs"], RQ["gd"], ALU.add)
            rcp(RQ["gw"], RQ["gs"])
            ts("dve", RQ["gw"], RQ["gw"], 1.0 / ALPHA, None, ALU.mult, None, rb_, rb_)
            ts("dve", RQ["pen"], RQ["goh"], -1.0, 1e30, ALU.add, ALU.mult, rb_, rb_)
            em4 = RQ["em"].rearrange("p t (g e) -> p t g e", e=4)
            tt("dve", em4, Ev.rearrange("p t (g e) -> p t g e", e=4), RQ["pen"].unsqueeze(3).broadcast_to([128, 16, 4, 4]),
               ALU.add, [bLGA] + rb_, rb_)
            red(RQ["m1"], RQ["em"], ALU.max)
            tt("dve", RQ["oh1"], RQ["em"], bc(RQ["m1"], 16), ALU.is_ge, rb_, rb_)
            emf = RQ["em"].rearrange("p t e -> p (t e)")
            stt(emf, RQ["oh1"].rearrange("p t e -> p (t e)"), -1e30, emf, ALU.mult, ALU.add, rb_, rb_)
            red(RQ["m2"], RQ["em"], ALU.max)
            tt("dve", RQ["oh2"], RQ["em"], bc(RQ["m2"], 16), ALU.is_ge, rb_, rb_)
            tt("dve", RQ["dif"], RQ["m2"], RQ["m1"], ALU.subtract, rb_, rb_)
            act(RQ["ex"], RQ["dif"], AF.Exp, rb_, rb_)
            ts("dve", RQ["w1"], RQ["ex"], 1.0, None, ALU.add, None, rb_, rb_)
            rcp(RQ["w1"], RQ["w1"])
            tt("dve", RQ["w2"], RQ["ex"], RQ["w1"], ALU.mult, rb_, rb_)
            tt("dve", RQ["a1"], RQ["w1"], RQ["gw"], ALU.mult, rb_, rb_)
            tt("dve", RQ["a2"], RQ["w2"], RQ["gw"], ALU.mult, rb_, rb_)
            tt("dve", RQ["em"], RQ["oh1"], bc(RQ["a1"], 16), ALU.mult, rb_, rb_)
            tt("dve", RQ["oh2"], RQ["oh2"], bc(RQ["a2"], 16), ALU.mult, rb_, rb_)
            tt("dve", CW, RQ["em"], RQ["oh2"], ALU.add, rb_, [bCW])
            if s == 0:
                dump("x1t", X1T, [128, 8, S], BF16, [bX1T])
                dump("cw", CW, [128, 16, 16], F32, [bCW])
            ec = Carver([(74 * KB, 122 * KB), (154 * KB, 174 * KB)])
            WG = [ec.get([128, 8, 512], BF16) for _ in range(2)]
            WU = [ec.get([128, 8, 512], BF16) for _ in range(2)]
            WD = [ec.get([128, 4, 1024], BF16) for _ in range(2)]
            bWE = [Buf(), Buf()]
            sem_we = [seq_dsem(), seq_dsem()]
            bWED = [Buf(), Buf()]
            sem_wed = [seq_dsem(), seq_dsem()]

            def load_expert(e_, extra_w=()):
                w_ = e_ % 2
                ops_ = [dma("pool", WG[w_], wg_d[e_].rearrange("(kc p) f -> p kc f", p=128), (), [bWE[w_]] + list(extra_w), sem_we[w_]),
                        dma("pool", WU[w_], wu_d[e_].rearrange("(kc p) f -> p kc f", p=128), (), [bWE[w_]], sem_we[w_]),
                        dma("pool", WD[w_], wd_d[e_].rearrange("(kc p) f -> p kc f", p=128), (), [bWED[w_]], sem_wed[w_])]
                return ops_

            pre = load_expert(0, [bWO, bMRG]) + load_expert(1)
            for o_ in pre:
                pending_dma.remove(o_)
            barrier()

            SG = [ec.get([128, 512], BF16) for _ in range(2)]
            bSG = [Buf(), Buf()]
            HT = [ec.get([128, 4, 512], BF16) for _ in range(2)]
            bHT = [Buf(), Buf()]
            LNG2 = ec.get([128, 1024], F32)
            LNB2 = ec.get([128, 1024], F32)
            SP3 = stat_pool(ec, 2)
            bLN2 = Buf()
            sem_l2 = seq_dsem()
            dma("sync", LNG2, ln2g_d, (), [bLN2], sem_l2)
            dma("sync", LNB2, ln2b_d, (), [bLN2], sem_l2)
            gi = 0
            yi = 0
            for e_ in range(16):
                w_ = e_ % 2
                if e_ >= 2:
                    load_expert(e_)
                for tg in range(4):
                    tsl = slice(tg * 512, (tg + 1) * 512)
                    hk = (e_ * 4 + tg) % 2
                    for fc in range(4):
                        base = 2 * (gi % 2)
                        k_ = gi % 2
                        gi += 1
                        fsl = slice(fc * 128, (fc + 1) * 128)
                        for kc in range(8):
                            mm(PB[base], WG[w_][:, kc, fsl], X1T[:, kc, tsl], kc == 0, kc == 7, [bWE[w_], bX1T], PBB[base])
                        for kc in range(8):
                            mm(PB[base + 1], WU[w_][:, kc, fsl], X1T[:, kc, tsl], kc == 0, kc == 7, [bWE[w_], bX1T],
                               PBB[base + 1])
                        act(SG[k_], PB[base], AF.Silu, PBB[base], [bSG[k_]])
                        tt("dve", HT[hk][:, fc, :], SG[k_], PB[base + 1], ALU.mult, PBB[base + 1] + [bSG[k_]], [bHT[hk]])
                    for t4 in range(4):
                        t = tg * 4 + t4
                        yb = 4 + 2 * (yi % 2)
                        yi += 1
                        for half in range(2):
                            for fc in range(4):
                                mm(PB[yb + half], HT[hk][:, fc, t4 * 128:(t4 + 1) * 128],
                                   WD[w_][:, fc, half * 512:(half + 1) * 512], fc == 0, fc == 3, [bHT[hk], bWED[w_]],
                                   PBB[yb + half])
                            hsl = slice(half * 512, (half + 1) * 512)
                            stt(X1[:, t, hsl], PB[yb + half], CW[:, t, e_:e_ + 1], X1[:, t, hsl], ALU.mult, ALU.add,
                                PBB[yb + half] + [bX1[t], bCW], [bX1[t]])
                        if e_ == 15:
                            xt_ = X1[:, t, :]
                            ln_affine(xt_, bX1[t], SP3[t % 2], LNG2, LNB2, bLN2, eps=EPS / (ALPHA * ALPHA), use_pool=True)
                            out_ops.append(dma("sync", out_d[s, t * 128:(t + 1) * 128, :], xt_, [bX1[t]], (), out_sems[t]))
                            if t == 7 and s + 1 < NSEQ:
                                for o_ in load_xt(s + 1, sem_xtg, [bX1[q_] for q_ in range(8)]):
                                    pending_dma.remove(o_)
            barrier()

        P.finalize()
        with nc.Block() as block:
            @block.sync
            def _(h):
                P.emit("sync", h, psem)
                for os_ in out_sems:
                    h.wait_ge(os_.sem, os_.count)

            @block.scalar
            def _(h):
                P.emit("act", h, psem)

            @block.vector
            def _(h):
                P.emit("dve", h, psem)

            @block.gpsimd
            def _(h):
                P.emit("pool", h, psem)

            @block.tensor
            def _(h):
                P.emit("pe", h, psem)
    return nc


def _t5_bucket(rel):
    half = 16
    max_exact = 8
    side = np.where(rel > 0, half, 0)
    n = np.abs(rel)
    large = max_exact + (np.log(np.maximum(n, 1).astype(np.float32) / max_exact)
                         / math.log(1024 / max_exact) * (half - max_exact)).astype(np.int32)
    large = np.minimum(large, half - 1)
    return side + np.where(n < max_exact, n, large)


def _constants():
    half = 64
    inv = (10000.0 ** (-np.arange(half, dtype=np.float64) / half))
    ang = np.arange(S, dtype=np.float64)[None, :] * inv[:, None]
    cosT = np.concatenate([np.cos(ang), np.cos(ang)], axis=0).astype(np.float32)
    sinT = np.concatenate([np.sin(ang), -np.sin(ang)], axis=0).astype(np.float32)
    j = np.arange(128)[:, None]
    i = np.arange(128)[None, :]
    p1 = np.maximum(i - j, 0).astype(np.float32)
    p2 = np.maximum(j - i, 0).astype(np.float32)
    eye = np.eye(128, dtype=np.float32)
    posf = np.broadcast_to((np.arange(128) + 1).astype(np.float32)[None, :], (128, 128))
    posb = np.broadcast_to((128 - np.arange(128)).astype(np.float32)[None, :], (128, 128))
    pcol = np.stack([127 - np.arange(128), np.arange(128)], axis=1).astype(np.float32)
    cst = np.concatenate([p1, p2, eye, posf, posb, pcol], axis=1).astype(np.float32)
    return cosT, sinT, np.ascontiguousarray(cst)


def _bias_tables(rel_bias):
    ii = np.arange(128)[:, None]
    jj = np.arange(256)[None, :]
    delta = ii + 64 - jj
    inwin = np.abs(delta) <= 64
    tb = np.full((128, 24, 256), NEG, dtype=np.float32)
    for pi, d in enumerate(PATTERNS):
        bk = _t5_bucket(delta * d)
        for h in range(8):
            sel = rel_bias[bk, h]
            tb[:, pi * 8 + h, :] = np.where(inwin, sel, np.float32(NEG))
    return tb.reshape(128, 24 * 256)


_NC_CACHE = {}


def kernel(x, w_in, ret_decay_fwd, ret_decay_bwd, ret_gn_g, w_proj_ret, rel_bias, w_proj_attn, w_out,
           ln1_g, ln1_b, router_w_group, router_b_group, router_w_expert, router_b_expert,
           w_gate, w_up, w_down, ln2_g, ln2_b):
    f = lambda a: np.ascontiguousarray(np.asarray(a, dtype=np.float32))
    x = f(x)
    cosT, sinT, cst = _constants()
    rep = lambda v: np.ascontiguousarray(np.broadcast_to(f(v).reshape(1, -1), (128, f(v).size)))
    shared = {
        "w_in": f(w_in)[0], "w_proj_ret": f(w_proj_ret)[0], "w_proj_attn": f(w_proj_attn)[0], "w_out": f(w_out)[0],
        "w_gate": f(w_gate)[0], "w_up": f(w_up)[0], "w_down": f(w_down)[0],
        "dec": rep(np.concatenate([f(ret_decay_fwd)[0], f(ret_decay_bwd)[0]])),
        "gng": np.ascontiguousarray(f(ret_gn_g)[0].reshape(8, 128).T), "ln1g": rep(f(ln1_g)[0]), "ln1b": rep(f(ln1_b)[0]),
        "ln2g": rep(f(ln2_g)[0]), "ln2b": rep(f(ln2_b)[0]),
        "rw": np.ascontiguousarray(np.concatenate([f(router_w_group)[0], f(router_w_expert)[0]], axis=1)),
        "rb": rep(np.concatenate([f(router_b_group)[0], f(router_b_expert)[0]])),
        "tb": _bias_tables(f(rel_bias)),
        "cosT": cosT, "sinT": sinT, "cst": cst,
    }
    in_maps = []
    for c in range(NCORES):
        xs = x[NSEQ * c:NSEQ * (c + 1)]
        m = dict(shared)
        m["x"] = np.ascontiguousarray(xs)
        m["xT"] = np.ascontiguousarray(xs.transpose(0, 2, 1))
        in_maps.append(m)
    if "nc" not in _NC_CACHE:
        _NC_CACHE["nc"] = build_program()
    res = run_bass_kernel_spmd(_NC_CACHE["nc"], in_maps, core_ids=list(range(NCORES)))
    if DBG:
        LAST["res"] = res.results
    return np.concatenate([np.asarray(r["out"], dtype=np.float32) for r in res.results], axis=0)
```

```python
import math
from contextlib import ExitStack
import numpy as np
import concourse.bass as bass
import concourse.mybir as mybir
from concourse.bass_utils import run_bass_kernel_spmd

F32 = mybir.dt.float32
BF16 = mybir.dt.bfloat16
U8 = mybir.dt.uint8
AF = mybir.ActivationFunctionType
ALU = mybir.AluOpType
AX = mybir.AxisListType

NCORES = 8
S = 2048
D = 1024
NSEQ = 2
ALPHA = 2.0 ** 0.25
EPS = 1e-5
CK = 128.0 ** -0.5
LNCK = math.log(CK)
NEG = -1e30
PATTERNS = (1, 4, 16)
DSIZE = {F32: 4, BF16: 2, U8: 1}
DBG = False
LAST = {}


class Buf:
    __slots__ = ("w", "r", "excl")

    def __init__(self, excl=False):
        self.w = {}
        self.r = {}
        self.excl = excl


class DSem:
    def __init__(self, sem):
        self.sem = sem
        self.count = 0


class Op:
    __slots__ = ("eng", "fn", "deps", "signaled", "count", "dsem", "dval", "id")


ENGS = ("sync", "act", "dve", "pool", "pe")


class Prog:
    def __init__(self):
        self.ops = {e: [] for e in ENGS}
        self.last = {}
        self.n = 0

    def add(self, eng, fn, reads=(), writes=(), dsem=None, extra=()):
        o = Op()
        o.eng = eng
        o.fn = fn
        o.signaled = False
        o.count = None
        o.dsem = dsem
        o.dval = None
        o.id = self.n
        self.n += 1
        if dsem is not None:
            dsem.count += 16
            o.dval = dsem.count
        writes = list(writes) + [b for b in reads if b.excl]
        reads = [b for b in reads if not b.excl]
        deps = {}
        raw = set()
        for d in extra:
            deps[d.id] = d
            raw.add(d.id)
        for b in reads:
            for d in b.w.values():
                deps[d.id] = d
                raw.add(d.id)
        for b in writes:
            for d in b.r.values():
                deps[d.id] = d
            for d in b.w.values():
                deps[d.id] = d
        o.deps = []
        for d in deps.values():
            if d is o:
                continue
            if d.dsem is None:
                same = d.eng == eng and dsem is None
                if (not same) or (d.id in raw and eng != "pe"):
                    d.signaled = True
                    o.deps.append(d)
            else:
                if dsem is not None and d.dsem is dsem and d.id not in raw:
                    continue
                o.deps.append(d)
        key = eng if dsem is None else ("d", id(dsem))
        for b in reads:
            b.r[key] = o
        for b in writes:
            if b.r:
                b.r = {}
                b.w = {key: o}
            else:
                b.w[key] = o
        self.ops[eng].append(o)
        self.last[eng] = o
        return o

    def finalize(self):
        for e in ENGS:
            c = 0
            for o in self.ops[e]:
                if o.signaled and o.dsem is None:
                    c += 1
                    o.count = c

    def emit(self, eng, h, psem):
        waited = {}
        for o in self.ops[eng]:
            need = {}
            for d in o.deps:
                if d.dsem is not None:
                    k, sem, v = ("d", id(d.dsem)), d.dsem.sem, d.dval
                else:
                    k, sem, v = d.eng, psem[d.eng], d.count
                if k not in need or need[k][1] < v:
                    need[k] = (sem, v)
            for k, (sem, v) in need.items():
                if waited.get(k, 0) < v:
                    h.wait_ge(sem, v)
                    waited[k] = v
            ins = o.fn(h)
            if o.dsem is not None:
                ins.then_inc(o.dsem.sem, 16)
            elif o.signaled:
                ins.then_inc(psem[eng], 1)


def build_program():
    nc = bass.Bass("TRN2", target_bir_lowering=False)
    P = Prog()

    def din(name, shape, dt=F32):
        return nc.dram_tensor(name, list(shape), dt, kind="ExternalInput").ap()

    xT_d = din("xT", [NSEQ, D, S])
    x_d = din("x", [NSEQ, S, D])
    w_in_d = din("w_in", [D, 6656])
    wpr_d = din("w_proj_ret", [1024, 1024])
    wpa_d = din("w_proj_attn", [512, 1024])
    wout_d = din("w_out", [1024, 1024])
    wg_d = din("w_gate", [16, 1024, 512])
    wu_d = din("w_up", [16, 1024, 512])
    wd_d = din("w_down", [16, 512, 1024])
    dec_d = din("dec", [128, 8])
    gng_d = din("gng", [128, 8])
    ln1g_d = din("ln1g", [128, 1024])
    ln1b_d = din("ln1b", [128, 1024])
    ln2g_d = din("ln2g", [128, 1024])
    ln2b_d = din("ln2b", [128, 1024])
    rw_d = din("rw", [1024, 20])
    rb_d = din("rb", [128, 20])
    tb_d = din("tb", [128, 24 * 256])
    cos_d = din("cosT", [128, S])
    sin_d = din("sinT", [128, S])
    cst_d = din("cst", [128, 5 * 128 + 2])
    out_d = nc.dram_tensor("out", [NSEQ, S, D], F32, kind="ExternalOutput").ap()

    w_in_v = w_in_d.rearrange("(kc p) c -> p kc c", p=128)
    wpr_v = wpr_d.rearrange("(kc p) c -> p kc c", p=128)
    wpa_v = wpa_d.rearrange("(kc p) c -> p kc c", p=128)
    wout_v = wout_d.rearrange("(kc p) c -> p kc c", p=128)
    rw_v = rw_d.rearrange("(kc p) c -> p kc c", p=128)

    es = ExitStack()
    with es:
        ARENA_BYTES = 174 * 1024
        arena = es.enter_context(nc.sbuf_tensor("arena", [128, ARENA_BYTES], U8))[:, :]
        PB = [es.enter_context(nc.psum_tensor(f"pb{i}", [128, 512], F32))[:, :] for i in range(8)]
        PBB = [[Buf(excl=True)] for _ in range(8)]
        psem = {e: es.enter_context(nc.semaphore(f"prog_{e}")) for e in ENGS}
        dsems = []

        def new_dsem():
            s_ = DSem(es.enter_context(nc.semaphore(f"dma{len(dsems)}")))
            dsems.append(s_)
            return s_

        def view(off, shape, dt):
            n = 1
            for v_ in shape[1:]:
                n *= v_
            nb = n * DSIZE[dt]
            assert off + nb <= ARENA_BYTES, (off, nb)
            v = arena[:, off:off + nb].bitcast(dt)
            if len(shape) == 3:
                v = v.rearrange("p (a b) -> p a b", b=shape[2])
            elif len(shape) == 4:
                v = v.rearrange("p (a b c) -> p a b c", b=shape[2], c=shape[3])
            return v

        KB = 1024

        class Carver:
            def __init__(self, regions):
                self.regions = [list(r) for r in regions]

            def get(self, shape, dt):
                n = 1
                for v_ in shape[1:]:
                    n *= v_
                nb = (n * DSIZE[dt] + 31) // 32 * 32
                for r in self.regions:
                    if r[1] - r[0] >= nb:
                        off = r[0]
                        r[0] += nb
                        return view(off, shape, dt)
                raise RuntimeError(f"arena overflow {shape}")

        def mm(out, lhsT, rhs, start, stop, r, w):
            return P.add("pe", lambda e: e.matmul(out, lhsT=lhsT, rhs=rhs, start=start, stop=stop,
                                                  skip_group_check=True), r, w)

        def tr(out, in_, ident, r, w):
            return P.add("pe", lambda e: e.transpose(out, in_, ident), r, w)

        def act(out, in_, func, r, w, bias=None, scale=None, accum=None):
            kw = {}
            if bias is not None:
                kw["bias"] = bias
            if scale is not None:
                kw["scale"] = scale
            if accum is not None:
                kw["accum_out"] = accum
            return P.add("act", lambda e: e.activation(out=out, in_=in_, func=func, **kw), r, w)

        def tt(eng, out, a, b, op, r, w):
            return P.add(eng, lambda e: e.tensor_tensor(out=out, in0=a, in1=b, op=op), r, w)

        def ts(eng, out, a, s1, s2, op0, op1, r, w):
            if op1 is None:
                return P.add(eng, lambda e: e.tensor_scalar(out=out, in0=a, scalar1=s1, scalar2=None, op0=op0), r, w)
            return P.add(eng, lambda e: e.tensor_scalar(out=out, in0=a, scalar1=s1, scalar2=s2, op0=op0, op1=op1), r, w)

        def stt(out, a, s_, b, op0, op1, r, w):
            return P.add("dve", lambda e: e.scalar_tensor_tensor(out=out, in0=a, scalar=s_, in1=b, op0=op0, op1=op1), r, w)

        def cp(eng, out, in_, r, w):
            if eng == "act":
                return P.add("act", lambda e: e.activation(out=out, in_=in_, func=AF.Copy), r, w)
            return P.add(eng, lambda e: e.tensor_copy(out=out, in_=in_), r, w)

        def memset(eng, ap, val, w):
            return P.add(eng, lambda e: e.memset(ap, val), (), w)

        def dma(eng, out, in_, r, w, dsem):
            o = P.add(eng, lambda e: e.dma_start(out=out, in_=in_), r, w, dsem=dsem)
            pending_dma.append(o)
            return o

        pending_dma = []
        dbg_sem = [None]

        def dump(name, ap, shape, dt, reads):
            if not DBG:
                return
            if dbg_sem[0] is None:
                dbg_sem[0] = new_dsem()
            t = nc.dram_tensor("dbg_" + name, list(shape), dt, kind="ExternalOutput").ap()
            dma("sync", t, ap, reads, (), dbg_sem[0])

        def barrier():
            b = Buf()
            for o in pending_dma:
                key = ("d", id(o.dsem))
                if key not in b.w or b.w[key].dval < o.dval:
                    b.w[key] = o
            del pending_dma[:]
            for e in ENGS:
                ex = [P.last[e]] if (e in P.last and e != "pe" and P.last[e].dsem is None) else []
                P.add(e, lambda h: h.nop(), (), [b], extra=ex)
            for e in ENGS:
                P.add(e, lambda h: h.nop(), [b], ())

        pc = Carver([(0, 10 * KB)])
        LG = pc.get([128, 8], F32)
        CD = pc.get([128, 8], F32)
        ZF = pc.get([128, 4], F32)
        ZB = pc.get([128, 4], F32)
        CONS = pc.get([128, 8], F32)
        DT = pc.get([128, 4, 128], F32)
        XIF = pc.get([128, 4, 128], F32)
        XIB = pc.get([128, 4, 128], F32)
        IDB = pc.get([128, 128], BF16)
        IDF = pc.get([128, 128], F32)
        RW = pc.get([128, 8, 20], F32)
        RB = pc.get([128, 20], F32)
        CW = pc.get([128, 16, 16], F32)
        ZERO = pc.get([128, 512], BF16)
        bPers = Buf()
        bCW = Buf()

        XT = view(10 * KB, [128, 8, S], BF16)
        OGT = view(42 * KB, [128, 8, S], BF16)
        ATT = view(74 * KB, [128, 4, S], BF16)
        MRG = view(90 * KB, [128, 8, S], BF16)
        X1 = view(10 * KB, [128, 16, 1024], F32)
        X1T = view(122 * KB, [128, 8, S], BF16)
        bXT, bOGT, bATT, bMRG, bX1T = Buf(), Buf(), Buf(), Buf(), Buf()
        bX1 = [Buf() for _ in range(16)]

        def load_xt(s_, sems_, extra_w=(), tgs=(0, 1, 2, 3)):
            xT_v = xT_d[s_].rearrange("(kc p) t -> p kc t", p=128)
            ops_ = []
            for tg in tgs:
                ops_.append(dma("pool", XT[:, :, tg * 512:(tg + 1) * 512], xT_v[:, :, tg * 512:(tg + 1) * 512], (),
                                [bXTg[tg]] + (list(extra_w) if tg == 0 else []), sems_[tg]))
            return ops_

        bXTg = [Buf() for _ in range(4)]
        sem_xtg = [new_dsem() for _ in range(4)]
        for o_ in load_xt(0, sem_xtg, tgs=(0,)):
            pending_dma.remove(o_)
        sc = Carver([(90 * KB, 170 * KB)])
        CST = sc.get([128, 642], F32)
        DEC = sc.get([128, 8], F32)
        TA = sc.get([128, 128], F32)
        TBm = sc.get([128, 128], F32)
        T8 = sc.get([128, 8], F32)
        sem_set = new_dsem()
        bset = Buf()
        dma("sync", CST, cst_d, (), [bset], sem_set)
        dma("sync", DEC, dec_d, (), [bset], sem_set)
        sem_set2 = new_dsem()
        dma("sync", RW, rw_v, (), [bPers], sem_set2)
        dma("sync", RB, rb_d, (), [bPers], sem_set2)
        P1 = CST[:, 0:128]
        P2 = CST[:, 128:256]
        EYE = CST[:, 256:384]
        POSF = CST[:, 384:512]
        POSB = CST[:, 512:640]
        PCOL = CST[:, 640:642]
        memset("pool", CONS[:, 0:1], 1.0, [bPers])
        memset("pool", CONS[:, 1:2], LNCK, [bPers])
        memset("pool", CONS[:, 2:3], EPS, [bPers])
        memset("pool", CONS[:, 3:4], EPS / (ALPHA * ALPHA), [bPers])
        memset("pool", CONS[:, 4:5], -0.5, [bPers])
        memset("pool", ZERO, 0.0, [bPers])
        cp("dve", IDB, EYE, [bset], [bPers])
        cp("dve", IDF, EYE, [bset], [bPers])
        act(T8, DEC, AF.Exp, [bset], [bset], scale=-1.0)
        act(T8, T8, AF.Ln, [bset, bPers], [bset], bias=CONS[:, 0:1])
        ts("dve", LG, T8, -1.0, None, ALU.mult, None, [bset], [bPers])
        act(CD, LG, AF.Exp, [bPers], [bPers], scale=128.0)
        for h in range(4):
            ts("dve", TA, P1, LG[:, h:h + 1], None, ALU.mult, None, [bset, bPers], [bset])
            stt(TBm, P2, LG[:, 4 + h:5 + h], TA, ALU.mult, ALU.add, [bset, bPers], [bset])
            act(DT[:, h, :], TBm, AF.Exp, [bset, bPers], [bPers], bias=CONS[:, 1:2])
            stt(DT[:, h, :], EYE, CK, DT[:, h, :], ALU.mult, ALU.add, [bset, bPers], [bPers])
            act(XIF[:, h, :], POSF, AF.Exp, [bset, bPers], [bPers], scale=LG[:, h:h + 1])
            act(XIB[:, h, :], POSB, AF.Exp, [bset, bPers], [bPers], scale=LG[:, 4 + h:5 + h])
            act(ZF[:, h:h + 1], PCOL[:, 0:1], AF.Exp, [bset, bPers], [bPers], scale=LG[:, h:h + 1], bias=CONS[:, 1:2])
            act(ZB[:, h:h + 1], PCOL[:, 1:2], AF.Exp, [bset, bPers], [bPers], scale=LG[:, 4 + h:5 + h], bias=CONS[:, 1:2])
        dump("lg", LG, [128, 8], F32, [bPers])
        dump("cd", CD, [128, 8], F32, [bPers])
        dump("zf", ZF, [128, 4], F32, [bPers])
        dump("dt", DT, [128, 4, 128], F32, [bPers])
        dump("xif", XIF, [128, 4, 128], F32, [bPers])
        barrier()

        out_sems = [new_dsem() for _ in range(16)]
        out_ops = []
        sem_x = [new_dsem() for _ in range(16)]

        def stat_pool(carver, n):
            return [dict(st=carver.get([128, 12], F32), mv=carver.get([128, 2], F32), ve=carver.get([128, 1], F32),
                         rs=carver.get([128, 1], F32), nm=carver.get([128, 1], F32), b=Buf(), b2=Buf()) for _ in range(n)]

        def norm_stats(src_aps, src_bufs, sp, eps=EPS, need_nm=False, use_pool=False):
            b = sp["b"]
            for i, a in enumerate(src_aps):
                P.add("dve", (lambda a_, o_: (lambda e: e.bn_stats(out=o_, in_=a_)))(a, sp["st"][:, 6 * i:6 * i + 6]),
                      src_bufs, [b])
            nst = len(src_aps) * 6
            P.add("dve", lambda e: e.bn_aggr(out=sp["mv"], in_=sp["st"][:, 0:nst]), [b], [b])
            b2 = sp["b2"]
            if use_pool:
                ts("dve", sp["ve"], sp["mv"][:, 1:2], eps, None, ALU.add, None, [b], [b2])
                tt("pool", sp["rs"], sp["ve"], CONS[:, 4:5], ALU.pow, [b2, bPers], [b2])
            else:
                act(sp["ve"], sp["mv"][:, 1:2], AF.Ln, [b, bPers], [b2], bias=(CONS[:, 2:3] if eps == EPS else CONS[:, 3:4]))
                act(sp["rs"], sp["ve"], AF.Exp, [b2], [b2], scale=-0.5)
            if need_nm:
                ts("dve", sp["nm"], sp["mv"][:, 0:1], sp["rs"], -1.0, ALU.mult, ALU.mult, [b, b2], [b2])

        def ln_affine(tile_ap, tb_, sp, G, Bv, bG, eps=EPS, use_pool=False):
            norm_stats([tile_ap[:, 0:512], tile_ap[:, 512:1024]], [tb_], sp, eps=eps, use_pool=use_pool)
            stt(tile_ap, tile_ap, sp["mv"][:, 0:1], G, ALU.subtract, ALU.mult, [tb_, sp["b"], bG], [tb_])
            stt(tile_ap, tile_ap, sp["rs"], Bv, ALU.mult, ALU.add, [tb_, sp["b2"], bG], [tb_])

        seq_pool = []
        seq_idx = [0]

        def seq_dsem():
            i_ = seq_idx[0]
            seq_idx[0] += 1
            if i_ == len(seq_pool):
                seq_pool.append(new_dsem())
            return seq_pool[i_]

        for s in range(NSEQ):
            seq_idx[0] = 0
            rc = Carver([(74 * KB, 174 * KB)])
            COS = rc.get([128, S], F32)
            SINX = rc.get([128, S], F32)
            GNG = rc.get([128, 8], F32)
            WH = rc.get([128, 8, 768], BF16)
            QROT = rc.get([128, S], BF16)
            KROT = rc.get([128, S], BF16)
            QXF = rc.get([128, S], BF16)
            QXB = rc.get([128, S], BF16)
            KZF = rc.get([128, 16, 128], BF16)
            KZB = rc.get([128, 16, 128], BF16)
            V = rc.get([128, 16, 256], BF16)
            GG = rc.get([128, 16, 256], BF16)
            SFALL = rc.get([128, 16, 256], BF16)
            SBALL = rc.get([128, 16, 256], BF16)
            SF32 = rc.get([128, 256], F32)
            SB32 = rc.get([128, 256], F32)
            T1 = [rc.get([128, 512], F32) for _ in range(2)]
            T2 = [rc.get([128, 512], F32) for _ in range(2)]
            bT = [Buf(), Buf()]
            STM = [rc.get([128, 128], BF16) for _ in range(3)]
            bSTM = [Buf() for _ in range(3)]
            ON = [rc.get([128, 256], F32) for _ in range(2)]
            OG = [rc.get([128, 256], BF16) for _ in range(4)]
            bON = [Buf(), Buf()]
            bOG = [Buf() for _ in range(4)]
            SP = stat_pool(rc, 4)
            bRC, bWH, bQ, bK, bQX, bKZ, bV, bGG, bSA, bS32, bS32B = (Buf() for _ in range(11))

            sem_r = seq_dsem()
            sem_xt = seq_dsem()
            sem_wh = seq_dsem()
            dma("sync", COS, cos_d, (), [bRC], sem_r)
            dma("sync", SINX, sin_d, (), [bRC], sem_r)
            dma("sync", GNG, gng_d, (), [bRC], sem_r)

            hs = [0]

            def halfslot(banks):
                bk = banks[hs[0] % len(banks)]
                hs[0] += 1
                return PB[bk][:, 0:256], PBB[bk]

            fb = [0]

            def fullbank(banks):
                bk = banks[fb[0] % len(banks)]
                fb[0] += 1
                return PB[bk], PBB[bk]

            rot = [0]

            def load_wh(h):
                for (c0, n_, d0) in ((h * 128, 128, 0), (512 + h * 128, 128, 128), (1024 + h * 256, 256, 256),
                                     (2048 + h * 256, 256, 512)):
                    dma("pool", WH[:, :, d0:d0 + n_], w_in_v[:, :, c0:c0 + n_], (), [bWH], sem_wh)

            load_wh(0)
            if s == 0:
                load_xt(0, sem_xtg, tgs=(1, 2, 3))
            for h in range(4):
                for which in range(2):
                    dst = QROT if which == 0 else KROT
                    bdst = bQ if which == 0 else bK
                    for tg in range(4):
                        bank, bb = fullbank([0, 1])
                        for kc in range(8):
                            mm(bank, WH[:, kc, which * 128:(which + 1) * 128], XT[:, kc, tg * 512:(tg + 1) * 512],
                               kc == 0, kc == 7, [bWH, bXTg[tg]], bb)
                        k_ = rot[0] % 2
                        rot[0] += 1
                        tsl = slice(tg * 512, (tg + 1) * 512)
                        tt("dve", T1[k_], bank, COS[:, tsl], ALU.mult, bb + [bRC], [bT[k_]])
                        tt("dve", T2[k_][0:64, :], bank[64:128, :], SINX[64:128, tsl], ALU.mult, bb + [bRC], [bT[k_]])
                        tt("dve", T2[k_][64:128, :], bank[0:64, :], SINX[0:64, tsl], ALU.mult, bb + [bRC], [bT[k_]])
                        tt("dve", dst[:, tsl], T1[k_], T2[k_], ALU.add, [bT[k_]], [bdst])
                q3 = QROT.rearrange("p (n c) -> p n c", c=128)
                tt("dve", QXF.rearrange("p (n c) -> p n c", c=128), q3,
                   XIF[:, h, :].unsqueeze(1).broadcast_to([128, 16, 128]), ALU.mult, [bQ, bPers], [bQX])
                tt("dve", QXB.rearrange("p (n c) -> p n c", c=128), q3,
                   XIB[:, h, :].unsqueeze(1).broadcast_to([128, 16, 128]), ALU.mult, [bQ, bPers], [bQX])
                memset("pool", SF32, 0.0, [bS32])
                memset("pool", SB32, 0.0, [bS32B])
                memset("pool", SFALL[:, 0, :], 0.0, [bSA])
                memset("pool", SBALL[:, 15, :], 0.0, [bSA])

                def ktr(n):
                    slot, sb = halfslot([2, 3])
                    sbf = slot.bitcast(BF16)
                    tr(sbf[:, 0:128], KROT[:, n * 128:(n + 1) * 128], IDB, [bK, bPers], sb)
                    act(KZF[:, n, :], sbf[:, 0:128], AF.Identity, sb + [bPers], [bKZ], scale=ZF[:, h:h + 1])
                    ts("dve", KZB[:, n, :], sbf[:, 0:128], ZB[:, h:h + 1], None, ALU.mult, None, sb + [bPers], [bKZ])

                def vg(n):
                    bank, bb = fullbank([0, 1])
                    for kc in range(8):
                        mm(bank, XT[:, kc, n * 128:(n + 1) * 128], WH[:, kc, 256:768], kc == 0, kc == 7, [bWH, bXTg[n // 4]], bb)
                    cp("dve", V[:, n, :], bank[:, 0:256], bb, [bV])
                    act(GG[:, n, :], bank[:, 256:512], AF.Silu, bb, [bGG])

                def fstep(n):
                    slot, sb = halfslot([2, 3])
                    mm(slot, KZF[:, n, :], V[:, n, :], True, True, [bKZ, bV], sb)
                    stt(SF32, SF32, CD[:, h:h + 1], slot, ALU.mult, ALU.add, sb + [bS32, bPers], [bS32])
                    cp("act", SFALL[:, n + 1, :], SF32, [bS32], [bSA])

                def bstep(m):
                    slot, sb = halfslot([2, 3])
                    mm(slot, KZB[:, m, :], V[:, m, :], True, True, [bKZ, bV], sb)
                    stt(SB32, SB32, CD[:, 4 + h:5 + h], slot, ALU.mult, ALU.add, sb + [bS32B, bPers], [bS32B])
                    cp("act", SBALL[:, m - 1, :], SB32, [bS32B], [bSA])

                def P_st2(j):
                    st_ = j % 2
                    kb = 2 + st_
                    kbf = PB[kb][:, 0:256].bitcast(BF16)
                    for q2, n in enumerate((j, 15 - j)):
                        tr(kbf[:, q2 * 128:(q2 + 1) * 128], KROT[:, n * 128:(n + 1) * 128], IDB, [bK, bPers], PBB[kb])
                    for q2, n in enumerate((j, 15 - j)):
                        vb = (0, 1, 4, 5)[2 * st_ + q2]
                        for kc in range(8):
                            mm(PB[vb], XT[:, kc, n * 128:(n + 1) * 128], WH[:, kc, 256:768], kc == 0, kc == 7,
                               [bWH, bXTg[n // 4]], PBB[vb])

                def E_st2(j):
                    st_ = j % 2
                    kb = 2 + st_
                    kbf = PB[kb][:, 0:256].bitcast(BF16)
                    for q2, n in enumerate((j, 15 - j)):
                        act(KZF[:, n, :], kbf[:, q2 * 128:(q2 + 1) * 128], AF.Identity, PBB[kb] + [bPers], [bKZ], scale=ZF[:, h:h + 1])
                        ts("dve", KZB[:, n, :], kbf[:, q2 * 128:(q2 + 1) * 128], ZB[:, h:h + 1], None, ALU.mult, None,
                           PBB[kb] + [bPers], [bKZ])
                    for q2, n in enumerate((j, 15 - j)):
                        vb = (0, 1, 4, 5)[2 * st_ + q2]
                        cp("dve", V[:, n, :], PB[vb][:, 0:256], PBB[vb], [bV])
                        act(GG[:, n, :], PB[vb][:, 256:512], AF.Silu, PBB[vb], [bGG])

                def SP_st2(j):
                    mm(PB[6][:, 0:256], KZF[:, j, :], V[:, j, :], True, True, [bKZ, bV], PBB[6])
                    mm(PB[7][:, 0:256], KZB[:, 15 - j, :], V[:, 15 - j, :], True, True, [bKZ, bV], PBB[7])

                def SE_st2(j):
                    stt(SF32, SF32, CD[:, h:h + 1], PB[6][:, 0:256], ALU.mult, ALU.add, PBB[6] + [bS32, bPers], [bS32])
                    cp("act", SFALL[:, j + 1, :], SF32, [bS32], [bSA])
                    stt(SB32, SB32, CD[:, 4 + h:5 + h], PB[7][:, 0:256], ALU.mult, ALU.add, PBB[7] + [bS32B, bPers], [bS32B])
                    cp("act", SBALL[:, 14 - j, :], SB32, [bS32B], [bSA])

                for i in range(8 + 2):
                    if 0 <= i - 1 < 8:
                        E_st2(i - 1)
                    if 0 <= i - 2 < 8:
                        SE_st2(i - 2)
                    if i < 8:
                        P_st2(i)
                    if 0 <= i - 1 < 8:
                        SP_st2(i - 1)
                if h < 3:
                    load_wh(h + 1)

                oorder = [7, 8]
                for k in range(1, 8):
                    oorder += [8 + k, 7 - k]

                slots = {}

                def P1(p):
                    n = oorder[p]
                    sl = slice(n * 128, (n + 1) * 128)
                    slot, sb = PB[p % 2][:, 0:256], PBB[p % 2]
                    slots[("s", p)] = (slot, sb)
                    mm(slot[:, 0:128], KROT[:, sl], QROT[:, sl], True, True, [bK, bQ], sb)

                def D1(p):
                    k3 = p % 3
                    slot, sb = slots.pop(("s", p))
                    tt("dve", STM[k3], slot[:, 0:128], DT[:, h, :], ALU.mult, sb + [bPers], [bSTM[k3]])

                def P2(p):
                    n = oorder[p]
                    sl = slice(n * 128, (n + 1) * 128)
                    k3 = p % 3
                    oslot, ob = PB[4 + p % 2][:, 0:256], PBB[4 + p % 2]
                    slots[("o", p)] = (oslot, ob)
                    mm(oslot, STM[k3], V[:, n, :], True, False, [bSTM[k3], bV], ob)
                    mm(oslot, QXF[:, sl], SFALL[:, n, :], False, False, [bQX, bSA], ob)
                    mm(oslot, QXB[:, sl], SBALL[:, n, :], False, True, [bQX, bSA], ob)

                def D2(p):
                    n = oorder[p]
                    k2 = p % 2
                    k4 = p % 4
                    oslot, ob = slots.pop(("o", p))
                    sp = SP[p % 4]
                    norm_stats([oslot], ob, sp)
                    stt(ON[k2], oslot, sp["mv"][:, 0:1], GG[:, n, :], ALU.subtract, ALU.mult, ob + [sp["b"], bGG], [bON[k2]])
                    act(OG[k4], ON[k2], AF.Identity, [bON[k2], sp["b2"]], [bOG[k4]], scale=sp["rs"])

                def P3(p):
                    k4 = p % 4
                    tslot, tb_ = PB[6 + p % 2][:, 0:256], PBB[6 + p % 2]
                    tbf = tslot.bitcast(BF16)
                    slots[("t", p)] = (tbf, tb_)
                    tr(tbf[:, 0:128], OG[k4][:, 0:128], IDB, [bOG[k4], bPers], tb_)
                    tr(tbf[:, 128:256], OG[k4][:, 128:256], IDB, [bOG[k4], bPers], tb_)

                def C3(p):
                    n = oorder[p]
                    sl = slice(n * 128, (n + 1) * 128)
                    tbf, tb_ = slots.pop(("t", p))
                    for j2 in range(2):
                        act(OGT[:, 2 * h + j2, sl], tbf[:, j2 * 128:(j2 + 1) * 128], AF.Identity, tb_ + [bRC], [bOGT],
                            scale=GNG[:, 2 * h + j2:2 * h + j2 + 1])

                for i in range(16 + 6):
                    if i < 16:
                        P1(i)
                    if 0 <= i - 1 < 16:
                        q_ = i - 1
                        D1(q_)
                        if q_ >= 2 and q_ % 2 == 0:
                            k = q_ // 2
                            fstep(7 + k)
                            bstep(8 - k)
                    if 0 <= i - 2 < 16:
                        P2(i - 2)
                    if 0 <= i - 3 < 16:
                        D2(i - 3)
                    if 0 <= i - 5 < 16:
                        P3(i - 5)
                    if 0 <= i - 6 < 16:
                        C3(i - 6)
                if s == 0:
                    dump(f"qrot{h}", QROT, [128, S], BF16, [bQ])
                    dump(f"krot{h}", KROT, [128, S], BF16, [bK])
                    dump(f"v{h}", V, [128, 16, 256], BF16, [bV])
                    dump(f"sfall{h}", SFALL, [128, 16, 256], BF16, [bSA])
                    dump(f"sball{h}", SBALL, [128, 16, 256], BF16, [bSA])
                    dump(f"qxf{h}", QXF, [128, S], BF16, [bQX])
                    dump(f"qxb{h}", QXB, [128, S], BF16, [bQX])
            if s == 0:
                dump("ogt", OGT, [128, 8, S], BF16, [bOGT])
                dump("xt", XT, [128, 8, S], BF16, bXTg)
            barrier()

            ac = Carver([(90 * KB, 174 * KB)])
            TB = ac.get([128, 24, 256], BF16)
            WA = ac.get([128, 8, 1536], BF16)
            QAM = [ac.get([128, S], BF16) for _ in range(2)]
            KA = ac.get([128, S], BF16)
            VT = ac.get([128, S], BF16)
            bVT = Buf()
            VAUG = ac.get([128, 96, 128], BF16)
            NPT = 4
            TMP = [ac.get([128, 256], F32) for _ in range(NPT)]
            PT = [ac.get([128, 256], BF16) for _ in range(NPT)]
            bTMP = [Buf() for _ in range(NPT)]
            bPT = [Buf() for _ in range(NPT)]
            ZR = [ac.get([128, 512], F32)] * 2
            bZR = [Buf()] * 2
            bTB, bWA, bQA, bKA, bVA = (Buf() for _ in range(5))
            sem_tb = seq_dsem()
            sem_wa = seq_dsem()
            bWA3 = [Buf(), Buf(), Buf()]
            sem_wa3 = [sem_wa, seq_dsem(), seq_dsem()]
            for j3 in range(3):
                dma("pool", WA[:, :, j3 * 512:(j3 + 1) * 512], w_in_v[:, :, 3072 + j3 * 512:3072 + (j3 + 1) * 512], (),
                    [bWA3[j3]], sem_wa3[j3])
            dma("pool", TB, tb_d.rearrange("p (a b) -> p a b", b=256), (), [bTB], sem_tb)
            memset("pool", VAUG[:, :, 64:128], 1.0, [bVA])
            memset("pool", QAM[0][64:128, :], 0.0, [bQA])
            memset("pool", QAM[1][0:64, :], 0.0, [bQA])
            for hp in range(4):
                for which in range(2):
                    for tg in range(4):
                        bank, bb = fullbank([4, 5])
                        c0 = which * 512 + hp * 128
                        for kc in range(8):
                            mm(bank, WA[:, kc, c0:c0 + 128], XT[:, kc, tg * 512:(tg + 1) * 512],
                               kc == 0, kc == 7, [bWA3[which], bXTg[tg]], bb)
                        tsl = slice(tg * 512, (tg + 1) * 512)
                        if which == 0:
                            act(QAM[0][0:64, tsl], bank[0:64, :], AF.Copy, bb, [bQA], scale=0.125)
                            act(QAM[1][64:128, tsl], bank[64:128, :], AF.Copy, bb, [bQA], scale=0.125)
                        else:
                            cp("act", KA[:, tsl], bank, bb, [bKA])
                for tg in range(4):
                    bank, bb = fullbank([4, 5])
                    for kc in range(8):
                        mm(bank, WA[:, kc, 1024 + hp * 128:1024 + (hp + 1) * 128], XT[:, kc, tg * 512:(tg + 1) * 512],
                           kc == 0, kc == 7, [bWA3[2], bXTg[tg]], bb)
                    cp("act", VT[:, tg * 512:(tg + 1) * 512], bank, bb, [bVT])
                for pi, d in enumerate(PATTERNS):
                    nsub = S // d
                    nb = nsub // 128
                    for r in range(d):
                        for b in range(nb):
                            blk = r * nb + b
                            slot, sb = halfslot([6, 7])
                            sbf = slot.bitcast(BF16)
                            vin = VT[:, :].rearrange("p (m r) -> p r m", r=d)[:, r, b * 128:(b + 1) * 128]
                            tr(sbf[:, 0:128], vin, IDB, [bVT, bPers], sb)
                            i0 = (pi * 16 + blk) * 2
                            cp("dve", VAUG[:, i0:i0 + 2, 0:64], sbf[:, 0:128].rearrange("p (a b) -> p a b", b=64), sb, [bVA])
                items = [(s2, pi, d, r, b) for s2 in range(2) for pi, d in enumerate(PATTERNS) for r in range(d)
                         for b in range(S // d // 128)]
                nper = len(items) // 2

                ast = {}

                def A1(i):
                    s2, pi, d, r, b = items[i]
                    nsub = S // d
                    KAv = KA[:, :].rearrange("p (m r) -> p r m", r=d)
                    QAv = QAM[s2][:, :].rearrange("p (m r) -> p r m", r=d)
                    a = 128 * b
                    q_lo = max(a - 64, 0)
                    q_hi = min(a + 192, nsub)
                    N = q_hi - q_lo
                    bk = 4 + i % 4
                    slot, sb = PB[bk][:, 0:256], PBB[bk]
                    ast[i] = (slot, sb)
                    mm(slot[:, 0:N], KAv[:, r, a:a + 128], QAv[:, r, q_lo:q_hi], True, True, [bKA, bQA], sb)

                def A1b(i):
                    s2, pi, d, r, b = items[i]
                    h = 2 * hp + s2
                    nsub = S // d
                    a = 128 * b
                    q_lo = max(a - 64, 0)
                    q_hi = min(a + 192, nsub)
                    N = q_hi - q_lo
                    j_lo = q_lo - (a - 64)
                    slot, sb = ast.pop(i)
                    k_ = i % NPT
                    tt("dve", TMP[k_][:, 0:N], slot[:, 0:N], TB[:, pi * 8 + h, j_lo:j_lo + N], ALU.add,
                       sb + [bTB], [bTMP[k_]])

                def A1c(i):
                    s2, pi, d, r, b = items[i]
                    nsub = S // d
                    a = 128 * b
                    N = min(a + 192, nsub) - max(a - 64, 0)
                    k_ = i % NPT
                    act(PT[k_][:, 0:N], TMP[k_][:, 0:N], AF.Exp, [bTMP[k_]], [bPT[k_]])

                def A2(i):
                    s2, pi, d, r, b = items[i]
                    prt = slice(64 * s2, 64 * s2 + 64)
                    nsub = S // d
                    nb = nsub // 128
                    blk = r * nb + b
                    a = 128 * b
                    q_lo = max(a - 64, 0)
                    q_hi = min(a + 192, nsub)
                    k_ = i % NPT
                    if i % nper == 0:
                        for k in range(4):
                            mm(PB[k], ZERO[:, 0:128], ZERO[:, 0:512], True, True, [bPers], PBB[k])
                    i0 = (pi * 16 + blk) * 2 + s2
                    per = 512 // d
                    for k in range(4):
                        m0 = max(q_lo, k * per)
                        m1 = min(q_hi, (k + 1) * per)
                        if m1 <= m0:
                            continue
                        ov = PB[k][:, :].rearrange("p (m r) -> p r m", r=d)[:, r, m0 - k * per:m1 - k * per]
                        mm(ov, VAUG[:, i0, :], PT[k_][:, m0 - q_lo:m1 - q_lo], False, False,
                           [bVA, bPT[k_]], PBB[k])
                    if i % nper == nper - 1:
                        for k in range(4):
                            z_ = k % 2
                            act(ZR[z_][64:128, :], PB[k][64:128, :], AF.Ln, PBB[k], [bZR[z_]])
                            act(ZR[z_][0:64, :], ZR[z_][64:128, :], AF.Exp, [bZR[z_]], [bZR[z_]], scale=-1.0)
                            tt("dve", ATT[prt, hp, k * 512:(k + 1) * 512], PB[k][0:64, :], ZR[z_][0:64, :], ALU.mult,
                               PBB[k] + [bZR[z_]], [bATT])

                nit = len(items)
                for i in range(nit + 4):
                    if i < nit:
                        A1(i)
                    if 0 <= i - 1 < nit:
                        A1b(i - 1)
                    if 0 <= i - 2 < nit:
                        A1c(i - 2)
                    if 0 <= i - 4 < nit:
                        A2(i - 4)
            if s == 0:
                dump("att", ATT, [128, 4, S], BF16, [bATT])
            barrier()

            mc = Carver([(122 * KB, 174 * KB)])
            WM = [mc.get([128, 28, 256], BF16) for _ in range(2)]
            bWM = [Buf(), Buf()]
            sem_wm = [seq_dsem(), seq_dsem()]
            SR = [mc.get([128, 512], F32) for _ in range(2)]
            SA_ = [mc.get([128, 512], F32) for _ in range(2)]
            M1 = [mc.get([128, 512], F32) for _ in range(2)]
            M2 = [mc.get([128, 512], F32) for _ in range(2)]
            bSR = [Buf(), Buf()]
            bSA2 = [Buf(), Buf()]
            bM1 = [Buf(), Buf()]
            bM2 = [Buf(), Buf()]
            it = 0
            def load_wm(c2):
                w_ = c2 % 2
                csl = slice(c2 * 256, (c2 + 1) * 256)
                dma("pool", WM[w_][:, 0:8, :], wpr_v[:, :, csl], (), [bWM[w_]], sem_wm[w_])
                dma("pool", WM[w_][:, 8:16, :], w_in_v[:, :, 4608 + c2 * 256:4608 + (c2 + 1) * 256], (), [bWM[w_]], sem_wm[w_])
                dma("pool", WM[w_][:, 16:20, :], wpa_v[:, :, csl], (), [bWM[w_]], sem_wm[w_])
                dma("pool", WM[w_][:, 20:28, :], w_in_v[:, :, 5632 + c2 * 256:5632 + (c2 + 1) * 256], (), [bWM[w_]], sem_wm[w_])

            load_wm(0)
            load_wm(1)
            for dc in range(8):
                c2 = dc // 2
                w_ = c2 % 2
                wsl = slice((dc % 2) * 128, (dc % 2) * 128 + 128)
                for tg in range(4):
                    tsl = slice(tg * 512, (tg + 1) * 512)
                    base = 4 * (it % 2)
                    k_ = it % 2
                    it += 1
                    bA, bB, bC, bD = base, base + 1, base + 2, base + 3
                    for kc in range(8):
                        mm(PB[bA], WM[w_][:, kc, wsl], OGT[:, kc, tsl], kc == 0, kc == 7, [bWM[w_], bOGT], PBB[bA])
                    for kc in range(8):
                        mm(PB[bB], WM[w_][:, 8 + kc, wsl], XT[:, kc, tsl], kc == 0, kc == 7, [bWM[w_], bXTg[tg]], PBB[bB])
                    for kc in range(4):
                        mm(PB[bC], WM[w_][:, 16 + kc, wsl], ATT[:, kc, tsl], kc == 0, kc == 3, [bWM[w_], bATT], PBB[bC])
                    for kc in range(8):
                        mm(PB[bD], WM[w_][:, 20 + kc, wsl], XT[:, kc, tsl], kc == 0, kc == 7, [bWM[w_], bXTg[tg]], PBB[bD])
                    act(SR[k_], PB[bB], AF.Sigmoid, PBB[bB], [bSR[k_]])
                    act(SA_[k_], PB[bD], AF.Sigmoid, PBB[bD], [bSA2[k_]])
                    tt("dve", M1[k_], SR[k_], PB[bA], ALU.mult, PBB[bA] + [bSR[k_]], [bM1[k_]])
                    tt("dve", M2[k_], SA_[k_], PB[bC], ALU.mult, PBB[bC] + [bSA2[k_]], [bM2[k_]])
                    tt("dve", MRG[:, dc, tsl], M1[k_], M2[k_], ALU.add, [bM1[k_], bM2[k_]], [bMRG])
                if dc % 2 == 1 and c2 + 2 < 4:
                    load_wm(c2 + 2)
            if s == 0:
                dump("mrg", MRG, [128, 8, S], BF16, [bMRG])
            barrier()

            m2c = Carver([(74 * KB, 90 * KB), (154 * KB, 174 * KB)])
            WOUT = m2c.get([128, 8, 1024], BF16)
            LNG = m2c.get([128, 1024], F32)
            LNB = m2c.get([128, 1024], F32)
            X1TF = m2c.get([128, 8, 128], F32)
            SP2 = stat_pool(m2c, 2)
            LGA = m2c.get([128, 16, 20], F32)
            RQ = dict((nm, m2c.get([128, 16], F32)) for nm in ("gm", "gs", "gw", "m1", "m2", "dif", "ex", "w1", "w2", "a1", "a2"))
            for nm in ("goh", "gd", "pen"):
                RQ[nm] = m2c.get([128, 16, 4], F32)
            for nm in ("em", "oh1", "oh2"):
                RQ[nm] = m2c.get([128, 16, 16], F32)
            bLGA, bRTB = Buf(), Buf()
            bWO, bLN, bX1TF = [Buf(), Buf()], Buf(), Buf()
            sem_m2 = seq_dsem()
            sem_wo = [seq_dsem(), seq_dsem()]
            for hf_ in range(2):
                dma("pool", WOUT[:, :, hf_ * 512:(hf_ + 1) * 512], wout_v[:, :, hf_ * 512:(hf_ + 1) * 512], (), [bWO[hf_]],
                    sem_wo[hf_])
            dma("sync", LNG, ln1g_d, (), [bLN], sem_m2)
            dma("sync", LNB, ln1b_d, (), [bLN], sem_m2)

            def M2a(t):
                sl = slice(t * 128, (t + 1) * 128)
                xt_ = X1[:, t, :]
                dma("sync", xt_, x_d[s, t * 128:(t + 1) * 128, :], (), [bX1[t]], sem_x[t])
                base = 2 * (t % 2)
                for half in range(2):
                    bk = base + half
                    for dc in range(8):
                        mm(PB[bk], MRG[:, dc, sl], WOUT[:, dc, half * 512:(half + 1) * 512], dc == 0, dc == 7,
                           [bMRG, bWO[half]], PBB[bk])
                    hsl = slice(half * 512, (half + 1) * 512)
                    stt(xt_[:, hsl], xt_[:, hsl], ALPHA, PB[bk], ALU.mult, ALU.add, PBB[bk] + [bX1[t]], [bX1[t]])
                ln_affine(xt_, bX1[t], SP2[t % 2], LNG, LNB, bLN)

            def M2b(t):
                sl = slice(t * 128, (t + 1) * 128)
                xt_ = X1[:, t, :]
                for g in range(2):
                    bk = 4 + g
                    for q in range(4):
                        dc = g * 4 + q
                        tr(PB[bk][:, q * 128:(q + 1) * 128], xt_[:, dc * 128:(dc + 1) * 128], IDF, [bX1[t], bPers], PBB[bk])
                    pv = PB[bk][:, :].rearrange("p (a b) -> p a b", b=128)
                    cp("act", X1T[:, g * 4:g * 4 + 4, sl], pv, PBB[bk], [bX1T])
                    cp("act", X1TF[:, g * 4:g * 4 + 4, :], pv, PBB[bk], [bX1TF])

            def M2c(t):
                for dc in range(8):
                    mm(PB[6][:, 0:20], X1TF[:, dc, :], RW[:, dc, :], dc == 0, dc == 7, [bX1TF, bPers], PBB[6])
                tt("dve", LGA[:, t, :], PB[6][:, 0:20], RB, ALU.add, PBB[6] + [bPers], [bLGA])

            for i in range(18):
                if i >= 2:
                    M2b(i - 2)
                if i < 16:
                    M2a(i)
                if i >= 2:
                    M2c(i - 2)
            rb_ = [bRTB]

            def bc(y2, k):
                return y2.unsqueeze(2).broadcast_to([128, 16, k])

            def red(out, in_, op):
                return P.add("dve", lambda e: e.tensor_reduce(out=out, in_=in_, axis=AX.X, op=op), [bLGA] + rb_, rb_)

            def rcp(out, in_):
                return P.add("dve", lambda e: e.reciprocal(out=out, in_=in_), rb_, rb_)

            Gv = LGA[:, :, 0:4]
            Ev = LGA[:, :, 4:20]
            red(RQ["gm"], Gv, ALU.max)
            tt("dve", RQ["goh"], Gv, bc(RQ["gm"], 4), ALU.is_ge, [bLGA] + rb_, rb_)
            tt("dve", RQ["gd"], Gv, bc(RQ["gm"], 4), ALU.subtract, [bLGA] + rb_, rb_)
            act(RQ["gd"], RQ["gd"], AF.Exp, rb_, rb_)
            red(RQ["gs"], RQ["gd"], ALU.add)
            rcp(RQ["gw"], RQ["gs"])
            ts("dve", RQ["gw"], RQ["gw"], 1.0 / ALPHA, None, ALU.mult, None, rb_, rb_)
            ts("dve", RQ["pen"], RQ["goh"], -1.0, 1e30, ALU.add, ALU.mult, rb_, rb_)
            em4 = RQ["em"].rearrange("p t (g e) -> p t g e", e=4)
            tt("dve", em4, Ev.rearrange("p t (g e) -> p t g e", e=4), RQ["pen"].unsqueeze(3).broadcast_to([128, 16, 4, 4]),
               ALU.add, [bLGA] + rb_, rb_)
            red(RQ["m1"], RQ["em"], ALU.max)
            tt("dve", RQ["oh1"], RQ["em"], bc(RQ["m1"], 16), ALU.is_ge, rb_, rb_)
            emf = RQ["em"].rearrange("p t e -> p (t e)")
            stt(emf, RQ["oh1"].rearrange("p t e -> p (t e)"), -1e30, emf, ALU.mult, ALU.add, rb_, rb_)
            red(RQ["m2"], RQ["em"], ALU.max)
            tt("dve", RQ["oh2"], RQ["em"], bc(RQ["m2"], 16), ALU.is_ge, rb_, rb_)
            tt("dve", RQ["dif"], RQ["m2"], RQ["m1"], ALU.subtract, rb_, rb_)
            act(RQ["ex"], RQ["dif"], AF.Exp, rb_, rb_)
            ts("dve", RQ["w1"], RQ["ex"], 1.0, None, ALU.add, None, rb_, rb_)
            rcp(RQ["w1"], RQ["w1"])
            tt("dve", RQ["w2"], RQ["ex"], RQ["w1"], ALU.mult, rb_, rb_)
            tt("dve", RQ["a1"], RQ["w1"], RQ["gw"], ALU.mult, rb_, rb_)
            tt("dve", RQ["a2"], RQ["w2"], RQ["gw"], ALU.mult, rb_, rb_)
            tt("dve", RQ["em"], RQ["oh1"], bc(RQ["a1"], 16), ALU.mult, rb_, rb_)
            tt("dve", RQ["oh2"], RQ["oh2"], bc(RQ["a2"], 16), ALU.mult, rb_, rb_)
            tt("dve", CW, RQ["em"], RQ["oh2"], ALU.add, rb_, [bCW])
            if s == 0:
                dump("x1t", X1T, [128, 8, S], BF16, [bX1T])
                dump("cw", CW, [128, 16, 16], F32, [bCW])
            ec = Carver([(74 * KB, 122 * KB), (154 * KB, 174 * KB)])
            WG = [ec.get([128, 8, 512], BF16) for _ in range(2)]
            WU = [ec.get([128, 8, 512], BF16) for _ in range(2)]
            WD = [ec.get([128, 4, 1024], BF16) for _ in range(2)]
            bWE = [Buf(), Buf()]
            sem_we = [seq_dsem(), seq_dsem()]
            bWED = [Buf(), Buf()]
            sem_wed = [seq_dsem(), seq_dsem()]

            def load_expert(e_, extra_w=()):
                w_ = e_ % 2
                ops_ = [dma("pool", WG[w_], wg_d[e_].rearrange("(kc p) f -> p kc f", p=128), (), [bWE[w_]] + list(extra_w), sem_we[w_]),
                        dma("pool", WU[w_], wu_d[e_].rearrange("(kc p) f -> p kc f", p=128), (), [bWE[w_]], sem_we[w_]),
                        dma("pool", WD[w_], wd_d[e_].rearrange("(kc p) f -> p kc f", p=128), (), [bWED[w_]], sem_wed[w_])]
                return ops_

            pre = load_expert(0, bWO + [bMRG]) + load_expert(1)
            for o_ in pre:
                pending_dma.remove(o_)
            barrier()

            SG = [ec.get([128, 512], BF16) for _ in range(2)]
            bSG = [Buf(), Buf()]
            HT = [ec.get([128, 4, 512], BF16) for _ in range(2)]
            bHT = [Buf(), Buf()]
            LNG2 = ec.get([128, 1024], F32)
            LNB2 = ec.get([128, 1024], F32)
            SP3 = stat_pool(ec, 2)
            bLN2 = Buf()
            sem_l2 = seq_dsem()
            dma("sync", LNG2, ln2g_d, (), [bLN2], sem_l2)
            dma("sync", LNB2, ln2b_d, (), [bLN2], sem_l2)
            gi = 0
            yi = 0
            for e_ in range(16):
                w_ = e_ % 2
                if e_ >= 2:
                    load_expert(e_)
                for tg in range(4):
                    tsl = slice(tg * 512, (tg + 1) * 512)
                    hk = (e_ * 4 + tg) % 2
                    for fc in range(4):
                        base = 2 * (gi % 2)
                        k_ = gi % 2
                        gi += 1
                        fsl = slice(fc * 128, (fc + 1) * 128)
                        for kc in range(8):
                            mm(PB[base], WG[w_][:, kc, fsl], X1T[:, kc, tsl], kc == 0, kc == 7, [bWE[w_], bX1T], PBB[base])
                        for kc in range(8):
                            mm(PB[base + 1], WU[w_][:, kc, fsl], X1T[:, kc, tsl], kc == 0, kc == 7, [bWE[w_], bX1T],
                               PBB[base + 1])
                        act(SG[k_], PB[base], AF.Silu, PBB[base], [bSG[k_]])
                        tt("dve", HT[hk][:, fc, :], SG[k_], PB[base + 1], ALU.mult, PBB[base + 1] + [bSG[k_]], [bHT[hk]])
                    for t4 in range(4):
                        t = tg * 4 + t4
                        yb = 4 + 2 * (yi % 2)
                        yi += 1
                        for half in range(2):
                            for fc in range(4):
                                mm(PB[yb + half], HT[hk][:, fc, t4 * 128:(t4 + 1) * 128],
                                   WD[w_][:, fc, half * 512:(half + 1) * 512], fc == 0, fc == 3, [bHT[hk], bWED[w_]],
                                   PBB[yb + half])
                            hsl = slice(half * 512, (half + 1) * 512)
                            stt(X1[:, t, hsl], PB[yb + half], CW[:, t, e_:e_ + 1], X1[:, t, hsl], ALU.mult, ALU.add,
                                PBB[yb + half] + [bX1[t], bCW], [bX1[t]])
                        if e_ == 15:
                            xt_ = X1[:, t, :]
                            ln_affine(xt_, bX1[t], SP3[t % 2], LNG2, LNB2, bLN2, eps=EPS / (ALPHA * ALPHA), use_pool=True)
                            out_ops.append(dma("sync", out_d[s, t * 128:(t + 1) * 128, :], xt_, [bX1[t]], (), out_sems[t]))
                            if t == 7 and s + 1 < NSEQ:
                                for o_ in load_xt(s + 1, sem_xtg, [bX1[q_] for q_ in range(8)]):
                                    pending_dma.remove(o_)
            barrier()

        P.finalize()
        with nc.Block() as block:
            @block.sync
            def _(h):
                P.emit("sync", h, psem)
                for os_ in out_sems:
                    h.wait_ge(os_.sem, os_.count)

            @block.scalar
            def _(h):
                P.emit("act", h, psem)

            @block.vector
            def _(h):
                P.emit("dve", h, psem)

            @block.gpsimd
            def _(h):
                P.emit("pool", h, psem)

            @block.tensor
            def _(h):
                P.emit("pe", h, psem)
    return nc


def _t5_bucket(rel):
    half = 16
    max_exact = 8
    side = np.where(rel > 0, half, 0)
    n = np.abs(rel)
    large = max_exact + (np.log(np.maximum(n, 1).astype(np.float32) / max_exact)
                         / math.log(1024 / max_exact) * (half - max_exact)).astype(np.int32)
    large = np.minimum(large, half - 1)
    return side + np.where(n < max_exact, n, large)


def _constants():
    half = 64
    inv = (10000.0 ** (-np.arange(half, dtype=np.float64) / half))
    ang = np.arange(S, dtype=np.float64)[None, :] * inv[:, None]
    cosT = np.concatenate([np.cos(ang), np.cos(ang)], axis=0).astype(np.float32)
    sinT = np.concatenate([np.sin(ang), -np.sin(ang)], axis=0).astype(np.float32)
    j = np.arange(128)[:, None]
    i = np.arange(128)[None, :]
    p1 = np.maximum(i - j, 0).astype(np.float32)
    p2 = np.maximum(j - i, 0).astype(np.float32)
    eye = np.eye(128, dtype=np.float32)
    posf = np.broadcast_to((np.arange(128) + 1).astype(np.float32)[None, :], (128, 128))
    posb = np.broadcast_to((128 - np.arange(128)).astype(np.float32)[None, :], (128, 128))
    pcol = np.stack([127 - np.arange(128), np.arange(128)], axis=1).astype(np.float32)
    cst = np.concatenate([p1, p2, eye, posf, posb, pcol], axis=1).astype(np.float32)
    return cosT, sinT, np.ascontiguousarray(cst)


def _bias_tables(rel_bias):
    ii = np.arange(128)[:, None]
    jj = np.arange(256)[None, :]
    delta = ii + 64 - jj
    inwin = np.abs(delta) <= 64
    tb = np.full((128, 24, 256), NEG, dtype=np.float32)
    for pi, d in enumerate(PATTERNS):
        bk = _t5_bucket(delta * d)
        for h in range(8):
            sel = rel_bias[bk, h]
            tb[:, pi * 8 + h, :] = np.where(inwin, sel, np.float32(NEG))
    return tb.reshape(128, 24 * 256)


_NC_CACHE = {}


def kernel(x, w_in, ret_decay_fwd, ret_decay_bwd, ret_gn_g, w_proj_ret, rel_bias, w_proj_attn, w_out,
           ln1_g, ln1_b, router_w_group, router_b_group, router_w_expert, router_b_expert,
           w_gate, w_up, w_down, ln2_g, ln2_b):
    f = lambda a: np.ascontiguousarray(np.asarray(a, dtype=np.float32))
    x = f(x)
    cosT, sinT, cst = _constants()
    rep = lambda v: np.ascontiguousarray(np.broadcast_to(f(v).reshape(1, -1), (128, f(v).size)))
    shared = {
        "w_in": f(w_in)[0], "w_proj_ret": f(w_proj_ret)[0], "w_proj_attn": f(w_proj_attn)[0], "w_out": f(w_out)[0],
        "w_gate": f(w_gate)[0], "w_up": f(w_up)[0], "w_down": f(w_down)[0],
        "dec": rep(np.concatenate([f(ret_decay_fwd)[0], f(ret_decay_bwd)[0]])),
        "gng": np.ascontiguousarray(f(ret_gn_g)[0].reshape(8, 128).T), "ln1g": rep(f(ln1_g)[0]), "ln1b": rep(f(ln1_b)[0]),
        "ln2g": rep(f(ln2_g)[0]), "ln2b": rep(f(ln2_b)[0]),
        "rw": np.ascontiguousarray(np.concatenate([f(router_w_group)[0], f(router_w_expert)[0]], axis=1)),
        "rb": rep(np.concatenate([f(router_b_group)[0], f(router_b_expert)[0]])),
        "tb": _bias_tables(f(rel_bias)),
        "cosT": cosT, "sinT": sinT, "cst": cst,
    }
    in_maps = []
    for c in range(NCORES):
        xs = x[NSEQ * c:NSEQ * (c + 1)]
        m = dict(shared)
        m["x"] = np.ascontiguousarray(xs)
        m["xT"] = np.ascontiguousarray(xs.transpose(0, 2, 1))
        in_maps.append(m)
    if "nc" not in _NC_CACHE:
        _NC_CACHE["nc"] = build_program()
    res = run_bass_kernel_spmd(_NC_CACHE["nc"], in_maps, core_ids=list(range(NCORES)))
    if DBG:
        LAST["res"] = res.results
    return np.concatenate([np.asarray(r["out"], dtype=np.float32) for r in res.results], axis=0)
```

```python
import math
from contextlib import ExitStack
import numpy as np
import concourse.bass as bass
import concourse.mybir as mybir
from concourse.bass_utils import run_bass_kernel_spmd

F32 = mybir.dt.float32
BF16 = mybir.dt.bfloat16
U8 = mybir.dt.uint8
AF = mybir.ActivationFunctionType
ALU = mybir.AluOpType
AX = mybir.AxisListType

NCORES = 8
S = 2048
D = 1024
NSEQ = 2
ALPHA = 2.0 ** 0.25
EPS = 1e-5
CK = 128.0 ** -0.5
LNCK = math.log(CK)
NEG = -1e30
PATTERNS = (1, 4, 16)
DSIZE = {F32: 4, BF16: 2, U8: 1}
DBG = False
LAST = {}


class Buf:
    __slots__ = ("w", "r", "excl")

    def __init__(self, excl=False):
        self.w = {}
        self.r = {}
        self.excl = excl


class DSem:
    def __init__(self, sem):
        self.sem = sem
        self.count = 0


class Op:
    __slots__ = ("eng", "fn", "deps", "signaled", "count", "dsem", "dval", "id")


ENGS = ("sync", "act", "dve", "pool", "pe")


class Prog:
    def __init__(self):
        self.ops = {e: [] for e in ENGS}
        self.last = {}
        self.n = 0

    def add(self, eng, fn, reads=(), writes=(), dsem=None, extra=()):
        o = Op()
        o.eng = eng
        o.fn = fn
        o.signaled = False
        o.count = None
        o.dsem = dsem
        o.dval = None
        o.id = self.n
        self.n += 1
        if dsem is not None:
            dsem.count += 16
            o.dval = dsem.count
        writes = list(writes) + [b for b in reads if b.excl]
        reads = [b for b in reads if not b.excl]
        deps = {}
        raw = set()
        for d in extra:
            deps[d.id] = d
            raw.add(d.id)
        for b in reads:
            for d in b.w.values():
                deps[d.id] = d
                raw.add(d.id)
        for b in writes:
            for d in b.r.values():
                deps[d.id] = d
            for d in b.w.values():
                deps[d.id] = d
        o.deps = []
        for d in deps.values():
            if d is o:
                continue
            if d.dsem is None:
                same = d.eng == eng and dsem is None
                if (not same) or (d.id in raw and eng != "pe"):
                    d.signaled = True
                    o.deps.append(d)
            else:
                if dsem is not None and d.dsem is dsem and d.id not in raw:
                    continue
                o.deps.append(d)
        key = eng if dsem is None else ("d", id(dsem))
        for b in reads:
            b.r[key] = o
        for b in writes:
            if b.r:
                b.r = {}
                b.w = {key: o}
            else:
                b.w[key] = o
        self.ops[eng].append(o)
        self.last[eng] = o
        return o

    def finalize(self):
        for e in ENGS:
            c = 0
            for o in self.ops[e]:
                if o.signaled and o.dsem is None:
                    c += 1
                    o.count = c

    def emit(self, eng, h, psem):
        waited = {}
        for o in self.ops[eng]:
            need = {}
            for d in o.deps:
                if d.dsem is not None:
                    k, sem, v = ("d", id(d.dsem)), d.dsem.sem, d.dval
                else:
                    k, sem, v = d.eng, psem[d.eng], d.count
                if k not in need or need[k][1] < v:
                    need[k] = (sem, v)
            for k, (sem, v) in need.items():
                if waited.get(k, 0) < v:
                    h.wait_ge(sem, v)
                    waited[k] = v
            ins = o.fn(h)
            if o.dsem is not None:
                ins.then_inc(o.dsem.sem, 16)
            elif o.signaled:
                ins.then_inc(psem[eng], 1)


def build_program():
    nc = bass.Bass("TRN2", target_bir_lowering=False)
    P = Prog()

    def din(name, shape, dt=F32):
        return nc.dram_tensor(name, list(shape), dt, kind="ExternalInput").ap()

    xT_d = din("xT", [NSEQ, D, S])
    x_d = din("x", [NSEQ, S, D])
    w_in_d = din("w_in", [D, 6656])
    wpr_d = din("w_proj_ret", [1024, 1024])
    wpa_d = din("w_proj_attn", [512, 1024])
    wout_d = din("w_out", [1024, 1024])
    wg_d = din("w_gate", [16, 1024, 512])
    wu_d = din("w_up", [16, 1024, 512])
    wd_d = din("w_down", [16, 512, 1024])
    dec_d = din("dec", [128, 8])
    gng_d = din("gng", [128, 8])
    ln1g_d = din("ln1g", [128, 1024])
    ln1b_d = din("ln1b", [128, 1024])
    ln2g_d = din("ln2g", [128, 1024])
    ln2b_d = din("ln2b", [128, 1024])
    rw_d = din("rw", [1024, 20])
    rb_d = din("rb", [128, 20])
    tb_d = din("tb", [128, 24 * 256])
    cos_d = din("cosT", [128, S])
    sin_d = din("sinT", [128, S])
    cst_d = din("cst", [128, 5 * 128 + 2])
    out_d = nc.dram_tensor("out", [NSEQ, S, D], F32, kind="ExternalOutput").ap()

    w_in_v = w_in_d.rearrange("(kc p) c -> p kc c", p=128)
    wpr_v = wpr_d.rearrange("(kc p) c -> p kc c", p=128)
    wpa_v = wpa_d.rearrange("(kc p) c -> p kc c", p=128)
    wout_v = wout_d.rearrange("(kc p) c -> p kc c", p=128)
    rw_v = rw_d.rearrange("(kc p) c -> p kc c", p=128)

    es = ExitStack()
    with es:
        ARENA_BYTES = 174 * 1024
        arena = es.enter_context(nc.sbuf_tensor("arena", [128, ARENA_BYTES], U8))[:, :]
        PB = [es.enter_context(nc.psum_tensor(f"pb{i}", [128, 512], F32))[:, :] for i in range(8)]
        PBB = [[Buf(excl=True)] for _ in range(8)]
        psem = {e: es.enter_context(nc.semaphore(f"prog_{e}")) for e in ENGS}
        dsems = []

        def new_dsem():
            s_ = DSem(es.enter_context(nc.semaphore(f"dma{len(dsems)}")))
            dsems.append(s_)
            return s_

        def view(off, shape, dt):
            n = 1
            for v_ in shape[1:]:
                n *= v_
            nb = n * DSIZE[dt]
            assert off + nb <= ARENA_BYTES, (off, nb)
            v = arena[:, off:off + nb].bitcast(dt)
            if len(shape) == 3:
                v = v.rearrange("p (a b) -> p a b", b=shape[2])
            elif len(shape) == 4:
                v = v.rearrange("p (a b c) -> p a b c", b=shape[2], c=shape[3])
            return v

        KB = 1024

        class Carver:
            def __init__(self, regions):
                self.regions = [list(r) for r in regions]

            def get(self, shape, dt):
                n = 1
                for v_ in shape[1:]:
                    n *= v_
                nb = (n * DSIZE[dt] + 31) // 32 * 32
                for r in self.regions:
                    if r[1] - r[0] >= nb:
                        off = r[0]
                        r[0] += nb
                        return view(off, shape, dt)
                raise RuntimeError(f"arena overflow {shape}")

        def mm(out, lhsT, rhs, start, stop, r, w):
            return P.add("pe", lambda e: e.matmul(out, lhsT=lhsT, rhs=rhs, start=start, stop=stop,
                                                  skip_group_check=True), r, w)

        def tr(out, in_, ident, r, w):
            return P.add("pe", lambda e: e.transpose(out, in_, ident), r, w)

        def act(out, in_, func, r, w, bias=None, scale=None, accum=None):
            kw = {}
            if bias is not None:
                kw["bias"] = bias
            if scale is not None:
                kw["scale"] = scale
            if accum is not None:
                kw["accum_out"] = accum
            return P.add("act", lambda e: e.activation(out=out, in_=in_, func=func, **kw), r, w)

        def tt(eng, out, a, b, op, r, w):
            return P.add(eng, lambda e: e.tensor_tensor(out=out, in0=a, in1=b, op=op), r, w)

        def ts(eng, out, a, s1, s2, op0, op1, r, w):
            if op1 is None:
                return P.add(eng, lambda e: e.tensor_scalar(out=out, in0=a, scalar1=s1, scalar2=None, op0=op0), r, w)
            return P.add(eng, lambda e: e.tensor_scalar(out=out, in0=a, scalar1=s1, scalar2=s2, op0=op0, op1=op1), r, w)

        def stt(out, a, s_, b, op0, op1, r, w):
            return P.add("dve", lambda e: e.scalar_tensor_tensor(out=out, in0=a, scalar=s_, in1=b, op0=op0, op1=op1), r, w)

        def cp(eng, out, in_, r, w):
            if eng == "act":
                return P.add("act", lambda e: e.activation(out=out, in_=in_, func=AF.Copy), r, w)
            return P.add(eng, lambda e: e.tensor_copy(out=out, in_=in_), r, w)

        def memset(eng, ap, val, w):
            return P.add(eng, lambda e: e.memset(ap, val), (), w)

        def dma(eng, out, in_, r, w, dsem):
            o = P.add(eng, lambda e: e.dma_start(out=out, in_=in_), r, w, dsem=dsem)
            pending_dma.append(o)
            return o

        pending_dma = []
        dbg_sem = [None]

        def dump(name, ap, shape, dt, reads):
            if not DBG:
                return
            if dbg_sem[0] is None:
                dbg_sem[0] = new_dsem()
            t = nc.dram_tensor("dbg_" + name, list(shape), dt, kind="ExternalOutput").ap()
            dma("sync", t, ap, reads, (), dbg_sem[0])

        def barrier():
            b = Buf()
            for o in pending_dma:
                key = ("d", id(o.dsem))
                if key not in b.w or b.w[key].dval < o.dval:
                    b.w[key] = o
            del pending_dma[:]
            for e in ENGS:
                ex = [P.last[e]] if (e in P.last and e != "pe" and P.last[e].dsem is None) else []
                P.add(e, lambda h: h.nop(), (), [b], extra=ex)
            for e in ENGS:
                P.add(e, lambda h: h.nop(), [b], ())

        pc = Carver([(0, 10 * KB)])
        LG = pc.get([128, 8], F32)
        CD = pc.get([128, 8], F32)
        ZF = pc.get([128, 4], F32)
        ZB = pc.get([128, 4], F32)
        CONS = pc.get([128, 8], F32)
        DT = pc.get([128, 4, 128], F32)
        XIF = pc.get([128, 4, 128], F32)
        XIB = pc.get([128, 4, 128], F32)
        IDB = pc.get([128, 128], BF16)
        IDF = pc.get([128, 128], F32)
        RW = pc.get([128, 8, 20], F32)
        RB = pc.get([128, 20], F32)
        CW = pc.get([128, 16, 16], F32)
        ZERO = pc.get([128, 512], BF16)
        bPers = Buf()
        bCW = Buf()

        XT = view(10 * KB, [128, 8, S], BF16)
        OGT = view(42 * KB, [128, 8, S], BF16)
        ATT = view(74 * KB, [128, 4, S], BF16)
        MRG = view(90 * KB, [128, 8, S], BF16)
        X1 = view(10 * KB, [128, 16, 1024], F32)
        X1T = view(122 * KB, [128, 8, S], BF16)
        bXT, bOGT, bATT, bMRG, bX1T = Buf(), Buf(), Buf(), Buf(), Buf()
        bX1 = [Buf() for _ in range(16)]

        def load_xt(s_, sems_, extra_w=(), tgs=(0, 1, 2, 3)):
            xT_v = xT_d[s_].rearrange("(kc p) t -> p kc t", p=128)
            ops_ = []
            for tg in tgs:
                ops_.append(dma("pool", XT[:, :, tg * 512:(tg + 1) * 512], xT_v[:, :, tg * 512:(tg + 1) * 512], (),
                                [bXTg[tg]] + (list(extra_w) if tg == 0 else []), sems_[tg]))
            return ops_

        bXTg = [Buf() for _ in range(4)]
        sem_xtg = [new_dsem() for _ in range(4)]
        for o_ in load_xt(0, sem_xtg, tgs=(0,)):
            pending_dma.remove(o_)
        sc = Carver([(90 * KB, 170 * KB)])
        CST = sc.get([128, 642], F32)
        DEC = sc.get([128, 8], F32)
        TA = sc.get([128, 128], F32)
        TBm = sc.get([128, 128], F32)
        T8 = sc.get([128, 8], F32)
        sem_set = new_dsem()
        bset = Buf()
        dma("sync", CST, cst_d, (), [bset], sem_set)
        dma("sync", DEC, dec_d, (), [bset], sem_set)
        sem_set2 = new_dsem()
        dma("sync", RW, rw_v, (), [bPers], sem_set2)
        dma("sync", RB, rb_d, (), [bPers], sem_set2)
        P1 = CST[:, 0:128]
        P2 = CST[:, 128:256]
        EYE = CST[:, 256:384]
        POSF = CST[:, 384:512]
        POSB = CST[:, 512:640]
        PCOL = CST[:, 640:642]
        memset("pool", CONS[:, 0:1], 1.0, [bPers])
        memset("pool", CONS[:, 1:2], LNCK, [bPers])
        memset("pool", CONS[:, 2:3], EPS, [bPers])
        memset("pool", CONS[:, 3:4], EPS / (ALPHA * ALPHA), [bPers])
        memset("pool", CONS[:, 4:5], -0.5, [bPers])
        memset("pool", ZERO, 0.0, [bPers])
        cp("dve", IDB, EYE, [bset], [bPers])
        cp("dve", IDF, EYE, [bset], [bPers])
        act(T8, DEC, AF.Exp, [bset], [bset], scale=-1.0)
        act(T8, T8, AF.Ln, [bset, bPers], [bset], bias=CONS[:, 0:1])
        ts("dve", LG, T8, -1.0, None, ALU.mult, None, [bset], [bPers])
        act(CD, LG, AF.Exp, [bPers], [bPers], scale=128.0)
        for h in range(4):
            ts("dve", TA, P1, LG[:, h:h + 1], None, ALU.mult, None, [bset, bPers], [bset])
            stt(TBm, P2, LG[:, 4 + h:5 + h], TA, ALU.mult, ALU.add, [bset, bPers], [bset])
            act(DT[:, h, :], TBm, AF.Exp, [bset, bPers], [bPers], bias=CONS[:, 1:2])
            stt(DT[:, h, :], EYE, CK, DT[:, h, :], ALU.mult, ALU.add, [bset, bPers], [bPers])
            act(XIF[:, h, :], POSF, AF.Exp, [bset, bPers], [bPers], scale=LG[:, h:h + 1])
            act(XIB[:, h, :], POSB, AF.Exp, [bset, bPers], [bPers], scale=LG[:, 4 + h:5 + h])
            act(ZF[:, h:h + 1], PCOL[:, 0:1], AF.Exp, [bset, bPers], [bPers], scale=LG[:, h:h + 1], bias=CONS[:, 1:2])
            act(ZB[:, h:h + 1], PCOL[:, 1:2], AF.Exp, [bset, bPers], [bPers], scale=LG[:, 4 + h:5 + h], bias=CONS[:, 1:2])
        dump("lg", LG, [128, 8], F32, [bPers])
        dump("cd", CD, [128, 8], F32, [bPers])
        dump("zf", ZF, [128, 4], F32, [bPers])
        dump("dt", DT, [128, 4, 128], F32, [bPers])
        dump("xif", XIF, [128, 4, 128], F32, [bPers])
        barrier()

        out_sems = [new_dsem() for _ in range(16)]
        out_ops = []
        sem_x = [new_dsem() for _ in range(16)]

        def stat_pool(carver, n):
            return [dict(st=carver.get([128, 12], F32), mv=carver.get([128, 2], F32), ve=carver.get([128, 1], F32),
                         rs=carver.get([128, 1], F32), nm=carver.get([128, 1], F32), b=Buf(), b2=Buf()) for _ in range(n)]

        def norm_stats(src_aps, src_bufs, sp, eps=EPS, need_nm=False, use_pool=False):
            b = sp["b"]
            for i, a in enumerate(src_aps):
                P.add("dve", (lambda a_, o_: (lambda e: e.bn_stats(out=o_, in_=a_)))(a, sp["st"][:, 6 * i:6 * i + 6]),
                      src_bufs, [b])
            nst = len(src_aps) * 6
            P.add("dve", lambda e: e.bn_aggr(out=sp["mv"], in_=sp["st"][:, 0:nst]), [b], [b])
            b2 = sp["b2"]
            if use_pool:
                ts("dve", sp["ve"], sp["mv"][:, 1:2], eps, None, ALU.add, None, [b], [b2])
                tt("pool", sp["rs"], sp["ve"], CONS[:, 4:5], ALU.pow, [b2, bPers], [b2])
            else:
                act(sp["ve"], sp["mv"][:, 1:2], AF.Ln, [b, bPers], [b2], bias=(CONS[:, 2:3] if eps == EPS else CONS[:, 3:4]))
                act(sp["rs"], sp["ve"], AF.Exp, [b2], [b2], scale=-0.5)
            if need_nm:
                ts("dve", sp["nm"], sp["mv"][:, 0:1], sp["rs"], -1.0, ALU.mult, ALU.mult, [b, b2], [b2])

        def ln_affine(tile_ap, tb_, sp, G, Bv, bG, eps=EPS, use_pool=False):
            norm_stats([tile_ap[:, 0:512], tile_ap[:, 512:1024]], [tb_], sp, eps=eps, use_pool=use_pool)
            stt(tile_ap, tile_ap, sp["mv"][:, 0:1], G, ALU.subtract, ALU.mult, [tb_, sp["b"], bG], [tb_])
            stt(tile_ap, tile_ap, sp["rs"], Bv, ALU.mult, ALU.add, [tb_, sp["b2"], bG], [tb_])

        seq_pool = []
        seq_idx = [0]

        def seq_dsem():
            i_ = seq_idx[0]
            seq_idx[0] += 1
            if i_ == len(seq_pool):
                seq_pool.append(new_dsem())
            return seq_pool[i_]

        for s in range(NSEQ):
            seq_idx[0] = 0
            rc = Carver([(74 * KB, 174 * KB)])
            COS = rc.get([128, S], F32)
            SINX = rc.get([128, S], F32)
            GNG = rc.get([128, 8], F32)
            WH = rc.get([128, 8, 768], BF16)
            QROT = rc.get([128, S], BF16)
            KROT = rc.get([128, S], BF16)
            QXF = rc.get([128, S], BF16)
            QXB = rc.get([128, S], BF16)
            KZF = rc.get([128, 16, 128], BF16)
            KZB = rc.get([128, 16, 128], BF16)
            V = rc.get([128, 16, 256], BF16)
            GG = rc.get([128, 16, 256], BF16)
            SFALL = rc.get([128, 16, 256], BF16)
            SBALL = rc.get([128, 16, 256], BF16)
            SF32 = rc.get([128, 256], F32)
            SB32 = rc.get([128, 256], F32)
            T1 = [rc.get([128, 512], F32) for _ in range(2)]
            T2 = [rc.get([128, 512], F32) for _ in range(2)]
            bT = [Buf(), Buf()]
            STM = [rc.get([128, 128], BF16) for _ in range(3)]
            bSTM = [Buf() for _ in range(3)]
            ON = [rc.get([128, 256], F32) for _ in range(2)]
            OG = [rc.get([128, 256], BF16) for _ in range(4)]
            bON = [Buf(), Buf()]
            bOG = [Buf() for _ in range(4)]
            SP = stat_pool(rc, 4)
            bRC, bWH, bQ, bK, bQX, bKZF, bV, bGG, bSA, bS32, bS32B = (Buf() for _ in range(11))
            bKZB = Buf()

            sem_r = seq_dsem()
            sem_xt = seq_dsem()
            sem_wh = seq_dsem()
            dma("sync", COS, cos_d, (), [bRC], sem_r)
            dma("sync", SINX, sin_d, (), [bRC], sem_r)
            dma("sync", GNG, gng_d, (), [bRC], sem_r)

            hs = [0]

            def halfslot(banks):
                bk = banks[hs[0] % len(banks)]
                hs[0] += 1
                return PB[bk][:, 0:256], PBB[bk]

            fb = [0]

            def fullbank(banks):
                bk = banks[fb[0] % len(banks)]
                fb[0] += 1
                return PB[bk], PBB[bk]

            rot = [0]

            def load_wh(h):
                for (c0, n_, d0) in ((h * 128, 128, 0), (512 + h * 128, 128, 128), (1024 + h * 256, 256, 256),
                                     (2048 + h * 256, 256, 512)):
                    dma("pool", WH[:, :, d0:d0 + n_], w_in_v[:, :, c0:c0 + n_], (), [bWH], sem_wh)

            load_wh(0)
            if s == 0:
                load_xt(0, sem_xtg, tgs=(1, 2, 3))
            for h in range(4):
                for which in range(2):
                    dst = QROT if which == 0 else KROT
                    bdst = bQ if which == 0 else bK
                    for tg in range(4):
                        bank, bb = fullbank([0, 1])
                        for kc in range(8):
                            mm(bank, WH[:, kc, which * 128:(which + 1) * 128], XT[:, kc, tg * 512:(tg + 1) * 512],
                               kc == 0, kc == 7, [bWH, bXTg[tg]], bb)
                        k_ = rot[0] % 2
                        rot[0] += 1
                        tsl = slice(tg * 512, (tg + 1) * 512)
                        tt("dve", T1[k_], bank, COS[:, tsl], ALU.mult, bb + [bRC], [bT[k_]])
                        tt("dve", T2[k_][0:64, :], bank[64:128, :], SINX[64:128, tsl], ALU.mult, bb + [bRC], [bT[k_]])
                        tt("dve", T2[k_][64:128, :], bank[0:64, :], SINX[0:64, tsl], ALU.mult, bb + [bRC], [bT[k_]])
                        tt("dve", dst[:, tsl], T1[k_], T2[k_], ALU.add, [bT[k_]], [bdst])
                q3 = QROT.rearrange("p (n c) -> p n c", c=128)
                tt("dve", QXF.rearrange("p (n c) -> p n c", c=128), q3,
                   XIF[:, h, :].unsqueeze(1).broadcast_to([128, 16, 128]), ALU.mult, [bQ, bPers], [bQX])
                tt("dve", QXB.rearrange("p (n c) -> p n c", c=128), q3,
                   XIB[:, h, :].unsqueeze(1).broadcast_to([128, 16, 128]), ALU.mult, [bQ, bPers], [bQX])
                memset("pool", SF32, 0.0, [bS32])
                memset("pool", SB32, 0.0, [bS32B])
                memset("pool", SFALL[:, 0, :], 0.0, [bSA])
                memset("pool", SBALL[:, 15, :], 0.0, [bSA])

                def ktr(n):
                    slot, sb = halfslot([2, 3])
                    sbf = slot.bitcast(BF16)
                    tr(sbf[:, 0:128], KROT[:, n * 128:(n + 1) * 128], IDB, [bK, bPers], sb)
                    act(KZF[:, n, :], sbf[:, 0:128], AF.Identity, sb + [bPers], [bKZF], scale=ZF[:, h:h + 1])
                    ts("dve", KZB[:, n, :], sbf[:, 0:128], ZB[:, h:h + 1], None, ALU.mult, None, sb + [bPers], [bKZB])

                def vg(n):
                    bank, bb = fullbank([0, 1])
                    for kc in range(8):
                        mm(bank, XT[:, kc, n * 128:(n + 1) * 128], WH[:, kc, 256:768], kc == 0, kc == 7, [bWH, bXTg[n // 4]], bb)
                    cp("dve", V[:, n, :], bank[:, 0:256], bb, [bV])
                    act(GG[:, n, :], bank[:, 256:512], AF.Silu, bb, [bGG])

                def fstep(n):
                    slot, sb = halfslot([2, 3])
                    mm(slot, KZF[:, n, :], V[:, n, :], True, True, [bKZF, bV], sb)
                    stt(SF32, SF32, CD[:, h:h + 1], slot, ALU.mult, ALU.add, sb + [bS32, bPers], [bS32])
                    cp("act", SFALL[:, n + 1, :], SF32, [bS32], [bSA])

                def bstep(m):
                    slot, sb = halfslot([2, 3])
                    mm(slot, KZB[:, m, :], V[:, m, :], True, True, [bKZB, bV], sb)
                    stt(SB32, SB32, CD[:, 4 + h:5 + h], slot, ALU.mult, ALU.add, sb + [bS32B, bPers], [bS32B])
                    cp("act", SBALL[:, m - 1, :], SB32, [bS32B], [bSA])

                def P_st2(j):
                    st_ = j % 2
                    kb = 2 + st_
                    kbf = PB[kb][:, 0:256].bitcast(BF16)
                    for q2, n in enumerate((j, 15 - j)):
                        tr(kbf[:, q2 * 128:(q2 + 1) * 128], KROT[:, n * 128:(n + 1) * 128], IDB, [bK, bPers], PBB[kb])
                    for q2, n in enumerate((j, 15 - j)):
                        vb = (0, 1, 4, 5)[2 * st_ + q2]
                        for kc in range(8):
                            mm(PB[vb], XT[:, kc, n * 128:(n + 1) * 128], WH[:, kc, 256:768], kc == 0, kc == 7,
                               [bWH, bXTg[n // 4]], PBB[vb])

                def E_st2(j):
                    st_ = j % 2
                    kb = 2 + st_
                    kbf = PB[kb][:, 0:256].bitcast(BF16)
                    for q2, n in enumerate((j, 15 - j)):
                        act(KZF[:, n, :], kbf[:, q2 * 128:(q2 + 1) * 128], AF.Identity, PBB[kb] + [bPers], [bKZF], scale=ZF[:, h:h + 1])
                        ts("dve", KZB[:, n, :], kbf[:, q2 * 128:(q2 + 1) * 128], ZB[:, h:h + 1], None, ALU.mult, None,
                           PBB[kb] + [bPers], [bKZB])
                    for q2, n in enumerate((j, 15 - j)):
                        vb = (0, 1, 4, 5)[2 * st_ + q2]
                        cp("dve", V[:, n, :], PB[vb][:, 0:256], PBB[vb], [bV])
                        act(GG[:, n, :], PB[vb][:, 256:512], AF.Silu, PBB[vb], [bGG])

                def SP_st2(j):
                    mm(PB[6][:, 0:256], KZF[:, j, :], V[:, j, :], True, True, [bKZF, bV], PBB[6])
                    mm(PB[7][:, 0:256], KZB[:, 15 - j, :], V[:, 15 - j, :], True, True, [bKZB, bV], PBB[7])

                def SE_st2(j):
                    stt(SF32, SF32, CD[:, h:h + 1], PB[6][:, 0:256], ALU.mult, ALU.add, PBB[6] + [bS32, bPers], [bS32])
                    cp("act", SFALL[:, j + 1, :], SF32, [bS32], [bSA])
                    stt(SB32, SB32, CD[:, 4 + h:5 + h], PB[7][:, 0:256], ALU.mult, ALU.add, PBB[7] + [bS32B, bPers], [bS32B])
                    cp("act", SBALL[:, 14 - j, :], SB32, [bS32B], [bSA])

                for i in range(8 + 2):
                    if 0 <= i - 1 < 8:
                        E_st2(i - 1)
                    if 0 <= i - 2 < 8:
                        SE_st2(i - 2)
                    if i < 8:
                        P_st2(i)
                    if 0 <= i - 1 < 8:
                        SP_st2(i - 1)
                if h < 3:
                    load_wh(h + 1)

                oorder = [7, 8]
                for k in range(1, 8):
                    oorder += [8 + k, 7 - k]

                slots = {}

                def P1(p):
                    n = oorder[p]
                    sl = slice(n * 128, (n + 1) * 128)
                    slot, sb = PB[p % 2][:, 0:256], PBB[p % 2]
                    slots[("s", p)] = (slot, sb)
                    mm(slot[:, 0:128], KROT[:, sl], QROT[:, sl], True, True, [bK, bQ], sb)

                def D1(p):
                    k3 = p % 3
                    slot, sb = slots.pop(("s", p))
                    tt("dve", STM[k3], slot[:, 0:128], DT[:, h, :], ALU.mult, sb + [bPers], [bSTM[k3]])

                def P2(p):
                    n = oorder[p]
                    sl = slice(n * 128, (n + 1) * 128)
                    k3 = p % 3
                    oslot, ob = PB[4 + p % 2][:, 0:256], PBB[4 + p % 2]
                    slots[("o", p)] = (oslot, ob)
                    mm(oslot, STM[k3], V[:, n, :], True, False, [bSTM[k3], bV], ob)
                    mm(oslot, QXF[:, sl], SFALL[:, n, :], False, False, [bQX, bSA], ob)
                    mm(oslot, QXB[:, sl], SBALL[:, n, :], False, True, [bQX, bSA], ob)

                def D2(p):
                    n = oorder[p]
                    k2 = p % 2
                    k4 = p % 4
                    oslot, ob = slots.pop(("o", p))
                    sp = SP[p % 4]
                    norm_stats([oslot], ob, sp)
                    stt(ON[k2], oslot, sp["mv"][:, 0:1], GG[:, n, :], ALU.subtract, ALU.mult, ob + [sp["b"], bGG], [bON[k2]])
                    act(OG[k4], ON[k2], AF.Identity, [bON[k2], sp["b2"]], [bOG[k4]], scale=sp["rs"])

                def P3(p):
                    k4 = p % 4
                    tslot, tb_ = PB[6 + p % 2][:, 0:256], PBB[6 + p % 2]
                    tbf = tslot.bitcast(BF16)
                    slots[("t", p)] = (tbf, tb_)
                    tr(tbf[:, 0:128], OG[k4][:, 0:128], IDB, [bOG[k4], bPers], tb_)
                    tr(tbf[:, 128:256], OG[k4][:, 128:256], IDB, [bOG[k4], bPers], tb_)

                def C3(p):
                    n = oorder[p]
                    sl = slice(n * 128, (n + 1) * 128)
                    tbf, tb_ = slots.pop(("t", p))
                    for j2 in range(2):
                        act(OGT[:, 2 * h + j2, sl], tbf[:, j2 * 128:(j2 + 1) * 128], AF.Identity, tb_ + [bRC], [bOGT],
                            scale=GNG[:, 2 * h + j2:2 * h + j2 + 1])

                for i in range(16 + 6):
                    if i < 16:
                        P1(i)
                    if 0 <= i - 1 < 16:
                        q_ = i - 1
                        D1(q_)
                        if q_ >= 2 and q_ % 2 == 0:
                            k = q_ // 2
                            fstep(7 + k)
                            bstep(8 - k)
                    if 0 <= i - 2 < 16:
                        P2(i - 2)
                    if 0 <= i - 3 < 16:
                        D2(i - 3)
                    if 0 <= i - 5 < 16:
                        P3(i - 5)
                    if 0 <= i - 6 < 16:
                        C3(i - 6)
                if s == 0:
                    dump(f"qrot{h}", QROT, [128, S], BF16, [bQ])
                    dump(f"krot{h}", KROT, [128, S], BF16, [bK])
                    dump(f"v{h}", V, [128, 16, 256], BF16, [bV])
                    dump(f"sfall{h}", SFALL, [128, 16, 256], BF16, [bSA])
                    dump(f"sball{h}", SBALL, [128, 16, 256], BF16, [bSA])
                    dump(f"qxf{h}", QXF, [128, S], BF16, [bQX])
                    dump(f"qxb{h}", QXB, [128, S], BF16, [bQX])
            if s == 0:
                dump("ogt", OGT, [128, 8, S], BF16, [bOGT])
                dump("xt", XT, [128, 8, S], BF16, bXTg)
            barrier()

            ac = Carver([(90 * KB, 174 * KB)])
            TB = ac.get([128, 24, 256], BF16)
            WA = ac.get([128, 8, 1536], BF16)
            QAM = [ac.get([128, S], BF16) for _ in range(2)]
            KA = ac.get([128, S], BF16)
            VT = ac.get([128, S], BF16)
            bVT = Buf()
            VAUG = ac.get([128, 96, 128], BF16)
            NPT = 4
            TMP = [ac.get([128, 256], F32) for _ in range(NPT)]
            PT = [ac.get([128, 256], BF16) for _ in range(NPT)]
            bTMP = [Buf() for _ in range(NPT)]
            bPT = [Buf() for _ in range(NPT)]
            ZR = [ac.get([128, 512], F32)] * 2
            bZR = [Buf()] * 2
            bTB, bWA, bQA, bKA, bVA = (Buf() for _ in range(5))
            sem_tb = seq_dsem()
            sem_wa = seq_dsem()
            bWA3 = [Buf(), Buf(), Buf()]
            sem_wa3 = [sem_wa, seq_dsem(), seq_dsem()]
            for j3 in range(3):
                dma("pool", WA[:, :, j3 * 512:(j3 + 1) * 512], w_in_v[:, :, 3072 + j3 * 512:3072 + (j3 + 1) * 512], (),
                    [bWA3[j3]], sem_wa3[j3])
            dma("pool", TB, tb_d.rearrange("p (a b) -> p a b", b=256), (), [bTB], sem_tb)
            memset("pool", VAUG[:, :, 64:128], 1.0, [bVA])
            memset("pool", QAM[0][64:128, :], 0.0, [bQA])
            memset("pool", QAM[1][0:64, :], 0.0, [bQA])
            for hp in range(4):
                for which in range(2):
                    for tg in range(4):
                        bank, bb = fullbank([4, 5])
                        c0 = which * 512 + hp * 128
                        for kc in range(8):
                            mm(bank, WA[:, kc, c0:c0 + 128], XT[:, kc, tg * 512:(tg + 1) * 512],
                               kc == 0, kc == 7, [bWA3[which], bXTg[tg]], bb)
                        tsl = slice(tg * 512, (tg + 1) * 512)
                        if which == 0:
                            act(QAM[0][0:64, tsl], bank[0:64, :], AF.Copy, bb, [bQA], scale=0.125)
                            act(QAM[1][64:128, tsl], bank[64:128, :], AF.Copy, bb, [bQA], scale=0.125)
                        else:
                            cp("act", KA[:, tsl], bank, bb, [bKA])
                for tg in range(4):
                    bank, bb = fullbank([4, 5])
                    for kc in range(8):
                        mm(bank, WA[:, kc, 1024 + hp * 128:1024 + (hp + 1) * 128], XT[:, kc, tg * 512:(tg + 1) * 512],
                           kc == 0, kc == 7, [bWA3[2], bXTg[tg]], bb)
                    cp("act", VT[:, tg * 512:(tg + 1) * 512], bank, bb, [bVT])
                for pi, d in enumerate(PATTERNS):
                    nsub = S // d
                    nb = nsub // 128
                    for r in range(d):
                        for b in range(nb):
                            blk = r * nb + b
                            slot, sb = halfslot([6, 7])
                            sbf = slot.bitcast(BF16)
                            vin = VT[:, :].rearrange("p (m r) -> p r m", r=d)[:, r, b * 128:(b + 1) * 128]
                            tr(sbf[:, 0:128], vin, IDB, [bVT, bPers], sb)
                            i0 = (pi * 16 + blk) * 2
                            cp("dve", VAUG[:, i0:i0 + 2, 0:64], sbf[:, 0:128].rearrange("p (a b) -> p a b", b=64), sb, [bVA])
                items = [(s2, pi, d, r, b) for s2 in range(2) for pi, d in enumerate(PATTERNS) for r in range(d)
                         for b in range(S // d // 128)]
                nper = len(items) // 2

                ast = {}

                def A1(i):
                    s2, pi, d, r, b = items[i]
                    nsub = S // d
                    KAv = KA[:, :].rearrange("p (m r) -> p r m", r=d)
                    QAv = QAM[s2][:, :].rearrange("p (m r) -> p r m", r=d)
                    a = 128 * b
                    q_lo = max(a - 64, 0)
                    q_hi = min(a + 192, nsub)
                    N = q_hi - q_lo
                    bk = 4 + i % 4
                    slot, sb = PB[bk][:, 0:256], PBB[bk]
                    ast[i] = (slot, sb)
                    mm(slot[:, 0:N], KAv[:, r, a:a + 128], QAv[:, r, q_lo:q_hi], True, True, [bKA, bQA], sb)

                def A1b(i):
                    s2, pi, d, r, b = items[i]
                    h = 2 * hp + s2
                    nsub = S // d
                    a = 128 * b
                    q_lo = max(a - 64, 0)
                    q_hi = min(a + 192, nsub)
                    N = q_hi - q_lo
                    j_lo = q_lo - (a - 64)
                    slot, sb = ast.pop(i)
                    k_ = i % NPT
                    tt("dve", TMP[k_][:, 0:N], slot[:, 0:N], TB[:, pi * 8 + h, j_lo:j_lo + N], ALU.add,
                       sb + [bTB], [bTMP[k_]])

                def A1c(i):
                    s2, pi, d, r, b = items[i]
                    nsub = S // d
                    a = 128 * b
                    N = min(a + 192, nsub) - max(a - 64, 0)
                    k_ = i % NPT
                    act(PT[k_][:, 0:N], TMP[k_][:, 0:N], AF.Exp, [bTMP[k_]], [bPT[k_]])

                def A2(i):
                    s2, pi, d, r, b = items[i]
                    prt = slice(64 * s2, 64 * s2 + 64)
                    nsub = S // d
                    nb = nsub // 128
                    blk = r * nb + b
                    a = 128 * b
                    q_lo = max(a - 64, 0)
                    q_hi = min(a + 192, nsub)
                    k_ = i % NPT
                    if i % nper == 0:
                        for k in range(4):
                            mm(PB[k], ZERO[:, 0:128], ZERO[:, 0:512], True, True, [bPers], PBB[k])
                    i0 = (pi * 16 + blk) * 2 + s2
                    per = 512 // d
                    for k in range(4):
                        m0 = max(q_lo, k * per)
                        m1 = min(q_hi, (k + 1) * per)
                        if m1 <= m0:
                            continue
                        ov = PB[k][:, :].rearrange("p (m r) -> p r m", r=d)[:, r, m0 - k * per:m1 - k * per]
                        mm(ov, VAUG[:, i0, :], PT[k_][:, m0 - q_lo:m1 - q_lo], False, False,
                           [bVA, bPT[k_]], PBB[k])
                    if i % nper == nper - 1:
                        for k in range(4):
                            z_ = k % 2
                            act(ZR[z_][64:128, :], PB[k][64:128, :], AF.Ln, PBB[k], [bZR[z_]])
                            act(ZR[z_][0:64, :], ZR[z_][64:128, :], AF.Exp, [bZR[z_]], [bZR[z_]], scale=-1.0)
                            tt("dve", ATT[prt, hp, k * 512:(k + 1) * 512], PB[k][0:64, :], ZR[z_][0:64, :], ALU.mult,
                               PBB[k] + [bZR[z_]], [bATT])

                nit = len(items)
                for i in range(nit + 4):
                    if i < nit:
                        A1(i)
                    if 0 <= i - 1 < nit:
                        A1b(i - 1)
                    if 0 <= i - 2 < nit:
                        A1c(i - 2)
                    if 0 <= i - 4 < nit:
                        A2(i - 4)
            if s == 0:
                dump("att", ATT, [128, 4, S], BF16, [bATT])
            barrier()

            mc = Carver([(122 * KB, 174 * KB)])
            WM = [mc.get([128, 28, 256], BF16) for _ in range(2)]
            bWM = [Buf(), Buf()]
            sem_wm = [seq_dsem(), seq_dsem()]
            SR = [mc.get([128, 512], F32) for _ in range(2)]
            SA_ = [mc.get([128, 512], F32) for _ in range(2)]
            M1 = [mc.get([128, 512], F32) for _ in range(2)]
            M2 = [mc.get([128, 512], F32) for _ in range(2)]
            bSR = [Buf(), Buf()]
            bSA2 = [Buf(), Buf()]
            bM1 = [Buf(), Buf()]
            bM2 = [Buf(), Buf()]
            it = 0
            def load_wm(c2):
                w_ = c2 % 2
                csl = slice(c2 * 256, (c2 + 1) * 256)
                dma("pool", WM[w_][:, 0:8, :], wpr_v[:, :, csl], (), [bWM[w_]], sem_wm[w_])
                dma("pool", WM[w_][:, 8:16, :], w_in_v[:, :, 4608 + c2 * 256:4608 + (c2 + 1) * 256], (), [bWM[w_]], sem_wm[w_])
                dma("pool", WM[w_][:, 16:20, :], wpa_v[:, :, csl], (), [bWM[w_]], sem_wm[w_])
                dma("pool", WM[w_][:, 20:28, :], w_in_v[:, :, 5632 + c2 * 256:5632 + (c2 + 1) * 256], (), [bWM[w_]], sem_wm[w_])

            load_wm(0)
            load_wm(1)
            for dc in range(8):
                c2 = dc // 2
                w_ = c2 % 2
                wsl = slice((dc % 2) * 128, (dc % 2) * 128 + 128)
                for tg in range(4):
                    tsl = slice(tg * 512, (tg + 1) * 512)
                    base = 4 * (it % 2)
                    k_ = it % 2
                    it += 1
                    bA, bB, bC, bD = base, base + 1, base + 2, base + 3
                    for kc in range(8):
                        mm(PB[bA], WM[w_][:, kc, wsl], OGT[:, kc, tsl], kc == 0, kc == 7, [bWM[w_], bOGT], PBB[bA])
                    for kc in range(8):
                        mm(PB[bB], WM[w_][:, 8 + kc, wsl], XT[:, kc, tsl], kc == 0, kc == 7, [bWM[w_], bXTg[tg]], PBB[bB])
                    for kc in range(4):
                        mm(PB[bC], WM[w_][:, 16 + kc, wsl], ATT[:, kc, tsl], kc == 0, kc == 3, [bWM[w_], bATT], PBB[bC])
                    for kc in range(8):
                        mm(PB[bD], WM[w_][:, 20 + kc, wsl], XT[:, kc, tsl], kc == 0, kc == 7, [bWM[w_], bXTg[tg]], PBB[bD])
                    act(SR[k_], PB[bB], AF.Sigmoid, PBB[bB], [bSR[k_]])
                    act(SA_[k_], PB[bD], AF.Sigmoid, PBB[bD], [bSA2[k_]])
                    tt("dve", M1[k_], SR[k_], PB[bA], ALU.mult, PBB[bA] + [bSR[k_]], [bM1[k_]])
                    tt("dve", M2[k_], SA_[k_], PB[bC], ALU.mult, PBB[bC] + [bSA2[k_]], [bM2[k_]])
                    tt("dve", MRG[:, dc, tsl], M1[k_], M2[k_], ALU.add, [bM1[k_], bM2[k_]], [bMRG])
                if dc % 2 == 1 and c2 + 2 < 4:
                    load_wm(c2 + 2)
            if s == 0:
                dump("mrg", MRG, [128, 8, S], BF16, [bMRG])
            barrier()

            m2c = Carver([(74 * KB, 90 * KB), (154 * KB, 174 * KB)])
            WOUT = m2c.get([128, 8, 1024], BF16)
            LNG = m2c.get([128, 1024], F32)
            LNB = m2c.get([128, 1024], F32)
            X1TF = m2c.get([128, 8, 128], F32)
            SP2 = stat_pool(m2c, 2)
            LGA = m2c.get([128, 16, 20], F32)
            RQ = dict((nm, m2c.get([128, 16], F32)) for nm in ("gm", "gs", "gw", "m1", "m2", "dif", "ex", "w1", "w2", "a1", "a2"))
            for nm in ("goh", "gd", "pen"):
                RQ[nm] = m2c.get([128, 16, 4], F32)
            for nm in ("em", "oh1", "oh2"):
                RQ[nm] = m2c.get([128, 16, 16], F32)
            bLGA, bRTB = Buf(), Buf()
            bWO, bLN, bX1TF = Buf(), Buf(), Buf()
            sem_m2 = seq_dsem()
            sem_wo = seq_dsem()
            dma("pool", WOUT, wout_v, (), [bWO], sem_wo)
            dma("sync", LNG, ln1g_d, (), [bLN], sem_m2)
            dma("sync", LNB, ln1b_d, (), [bLN], sem_m2)

            def M2a(t):
                sl = slice(t * 128, (t + 1) * 128)
                xt_ = X1[:, t, :]
                dma("sync", xt_, x_d[s, t * 128:(t + 1) * 128, :], (), [bX1[t]], sem_x[t])
                base = 2 * (t % 2)
                for half in range(2):
                    bk = base + half
                    for dc in range(8):
                        mm(PB[bk], MRG[:, dc, sl], WOUT[:, dc, half * 512:(half + 1) * 512], dc == 0, dc == 7,
                           [bMRG, bWO], PBB[bk])
                    hsl = slice(half * 512, (half + 1) * 512)
                    stt(xt_[:, hsl], xt_[:, hsl], ALPHA, PB[bk], ALU.mult, ALU.add, PBB[bk] + [bX1[t]], [bX1[t]])
                ln_affine(xt_, bX1[t], SP2[t % 2], LNG, LNB, bLN)

            def M2b(t):
                sl = slice(t * 128, (t + 1) * 128)
                xt_ = X1[:, t, :]
                for g in range(2):
                    bk = 4 + g
                    for q in range(4):
                        dc = g * 4 + q
                        tr(PB[bk][:, q * 128:(q + 1) * 128], xt_[:, dc * 128:(dc + 1) * 128], IDF, [bX1[t], bPers], PBB[bk])
                    pv = PB[bk][:, :].rearrange("p (a b) -> p a b", b=128)
                    cp("act", X1T[:, g * 4:g * 4 + 4, sl], pv, PBB[bk], [bX1T])
                    cp("act", X1TF[:, g * 4:g * 4 + 4, :], pv, PBB[bk], [bX1TF])

            def M2c(t):
                for dc in range(8):
                    mm(PB[6][:, 0:20], X1TF[:, dc, :], RW[:, dc, :], dc == 0, dc == 7, [bX1TF, bPers], PBB[6])
                tt("dve", LGA[:, t, :], PB[6][:, 0:20], RB, ALU.add, PBB[6] + [bPers], [bLGA])

            for i in range(18):
                if i >= 2:
                    M2b(i - 2)
                if i < 16:
                    M2a(i)
                if i >= 2:
                    M2c(i - 2)
            rb_ = [bRTB]

            def bc(y2, k):
                return y2.unsqueeze(2).broadcast_to([128, 16, k])

            def red(out, in_, op):
                return P.add("dve", lambda e: e.tensor_reduce(out=out, in_=in_, axis=AX.X, op=op), [bLGA] + rb_, rb_)

            def rcp(out, in_):
                return P.add("dve", lambda e: e.reciprocal(out=out, in_=in_), rb_, rb_)

            Gv = LGA[:, :, 0:4]
            Ev = LGA[:, :, 4:20]
            red(RQ["gm"], Gv, ALU.max)
            tt("dve", RQ["goh"], Gv, bc(RQ["gm"], 4), ALU.is_ge, [bLGA] + rb_, rb_)
            tt("dve", RQ["gd"], Gv, bc(RQ["gm"], 4), ALU.subtract, [bLGA] + rb_, rb_)
            act(RQ["gd"], RQ["gd"], AF.Exp, rb_, rb_)
            red(RQ["gs"], RQ["gd"], ALU.add)
            rcp(RQ["gw"], RQ["gs"])
            ts("dve", RQ["gw"], RQ["gw"], 1.0 / ALPHA, None, ALU.mult, None, rb_, rb_)
            ts("dve", RQ["pen"], RQ["goh"], -1.0, 1e30, ALU.add, ALU.mult, rb_, rb_)
            em4 = RQ["em"].rearrange("p t (g e) -> p t g e", e=4)
            tt("dve", em4, Ev.rearrange("p t (g e) -> p t g e", e=4), RQ["pen"].unsqueeze(3).broadcast_to([128, 16, 4, 4]),
               ALU.add, [bLGA] + rb_, rb_)
            red(RQ["m1"], RQ["em"], ALU.max)
            tt("dve", RQ["oh1"], RQ["em"], bc(RQ["m1"], 16), ALU.is_ge, rb_, rb_)
            emf = RQ["em"].rearrange("p t e -> p (t e)")
            stt(emf, RQ["oh1"].rearrange("p t e -> p (t e)"), -1e30, emf, ALU.mult, ALU.add, rb_, rb_)
            red(RQ["m2"], RQ["em"], ALU.max)
            tt("dve", RQ["oh2"], RQ["em"], bc(RQ["m2"], 16), ALU.is_ge, rb_, rb_)
            tt("dve", RQ["dif"], RQ["m2"], RQ["m1"], ALU.subtract, rb_, rb_)
            act(RQ["ex"], RQ["dif"], AF.Exp, rb_, rb_)
            ts("dve", RQ["w1"], RQ["ex"], 1.0, None, ALU.add, None, rb_, rb_)
            rcp(RQ["w1"], RQ["w1"])
            tt("dve", RQ["w2"], RQ["ex"], RQ["w1"], ALU.mult, rb_, rb_)
            tt("dve", RQ["a1"], RQ["w1"], RQ["gw"], ALU.mult, rb_, rb_)
            tt("dve", RQ["a2"], RQ["w2"], RQ["gw"], ALU.mult, rb_, rb_)
            tt("dve", RQ["em"], RQ["oh1"], bc(RQ["a1"], 16), ALU.mult, rb_, rb_)
            tt("dve", RQ["oh2"], RQ["oh2"], bc(RQ["a2"], 16), ALU.mult, rb_, rb_)
            tt("dve", CW, RQ["em"], RQ["oh2"], ALU.add, rb_, [bCW])
            if s == 0:
                dump("x1t", X1T, [128, 8, S], BF16, [bX1T])
                dump("cw", CW, [128, 16, 16], F32, [bCW])
            ec = Carver([(74 * KB, 122 * KB), (154 * KB, 174 * KB)])
            WG = [ec.get([128, 8, 512], BF16) for _ in range(2)]
            WU = [ec.get([128, 8, 512], BF16) for _ in range(2)]
            WD = [ec.get([128, 4, 1024], BF16) for _ in range(2)]
            bWE = [Buf(), Buf()]
            sem_we = [seq_dsem(), seq_dsem()]
            bWED = [Buf(), Buf()]
            sem_wed = [seq_dsem(), seq_dsem()]

            def load_expert(e_, extra_w=()):
                w_ = e_ % 2
                ops_ = [dma("pool", WG[w_], wg_d[e_].rearrange("(kc p) f -> p kc f", p=128), (), [bWE[w_]] + list(extra_w), sem_we[w_]),
                        dma("pool", WU[w_], wu_d[e_].rearrange("(kc p) f -> p kc f", p=128), (), [bWE[w_]], sem_we[w_]),
                        dma("pool", WD[w_], wd_d[e_].rearrange("(kc p) f -> p kc f", p=128), (), [bWED[w_]], sem_wed[w_])]
                return ops_

            pre = load_expert(0, [bWO, bMRG]) + load_expert(1)
            for o_ in pre:
                pending_dma.remove(o_)
            barrier()

            SG = [ec.get([128, 512], BF16) for _ in range(2)]
            bSG = [Buf(), Buf()]
            HT = [ec.get([128, 4, 512], BF16) for _ in range(2)]
            bHT = [Buf(), Buf()]
            LNG2 = ec.get([128, 1024], F32)
            LNB2 = ec.get([128, 1024], F32)
            SP3 = stat_pool(ec, 2)
            bLN2 = Buf()
            sem_l2 = seq_dsem()
            dma("sync", LNG2, ln2g_d, (), [bLN2], sem_l2)
            dma("sync", LNB2, ln2b_d, (), [bLN2], sem_l2)
            gi = 0
            yi = 0
            for e_ in range(16):
                w_ = e_ % 2
                if e_ >= 2:
                    load_expert(e_)
                for tg in range(4):
                    tsl = slice(tg * 512, (tg + 1) * 512)
                    hk = (e_ * 4 + tg) % 2
                    for fc in range(4):
                        base = 2 * (gi % 2)
                        k_ = gi % 2
                        gi += 1
                        fsl = slice(fc * 128, (fc + 1) * 128)
                        for kc in range(8):
                            mm(PB[base], WG[w_][:, kc, fsl], X1T[:, kc, tsl], kc == 0, kc == 7, [bWE[w_], bX1T], PBB[base])
                        for kc in range(8):
                            mm(PB[base + 1], WU[w_][:, kc, fsl], X1T[:, kc, tsl], kc == 0, kc == 7, [bWE[w_], bX1T],
                               PBB[base + 1])
                        act(SG[k_], PB[base], AF.Silu, PBB[base], [bSG[k_]])
                        tt("dve", HT[hk][:, fc, :], SG[k_], PB[base + 1], ALU.mult, PBB[base + 1] + [bSG[k_]], [bHT[hk]])
                    for t4 in range(4):
                        t = tg * 4 + t4
                        yb = 4 + 2 * (yi % 2)
                        yi += 1
                        for half in range(2):
                            for fc in range(4):
                                mm(PB[yb + half], HT[hk][:, fc, t4 * 128:(t4 + 1) * 128],
                                   WD[w_][:, fc, half * 512:(half + 1) * 512], fc == 0, fc == 3, [bHT[hk], bWED[w_]],
                                   PBB[yb + half])
                            hsl = slice(half * 512, (half + 1) * 512)
                            stt(X1[:, t, hsl], PB[yb + half], CW[:, t, e_:e_ + 1], X1[:, t, hsl], ALU.mult, ALU.add,
                                PBB[yb + half] + [bX1[t], bCW], [bX1[t]])
                        if e_ == 15:
                            xt_ = X1[:, t, :]
                            ln_affine(xt_, bX1[t], SP3[t % 2], LNG2, LNB2, bLN2, eps=EPS / (ALPHA * ALPHA), use_pool=True)
                            out_ops.append(dma("sync", out_d[s, t * 128:(t + 1) * 128, :], xt_, [bX1[t]], (), out_sems[t]))
                            if t == 7 and s + 1 < NSEQ:
                                for o_ in load_xt(s + 1, sem_xtg, [bX1[q_] for q_ in range(8)]):
                                    pending_dma.remove(o_)
            barrier()

        P.finalize()
        with nc.Block() as block:
            @block.sync
            def _(h):
                P.emit("sync", h, psem)
                for os_ in out_sems:
                    h.wait_ge(os_.sem, os_.count)

            @block.scalar
            def _(h):
                P.emit("act", h, psem)

            @block.vector
            def _(h):
                P.emit("dve", h, psem)

            @block.gpsimd
            def _(h):
                P.emit("pool", h, psem)

            @block.tensor
            def _(h):
                P.emit("pe", h, psem)
    return nc


def _t5_bucket(rel):
    half = 16
    max_exact = 8
    side = np.where(rel > 0, half, 0)
    n = np.abs(rel)
    large = max_exact + (np.log(np.maximum(n, 1).astype(np.float32) / max_exact)
                         / math.log(1024 / max_exact) * (half - max_exact)).astype(np.int32)
    large = np.minimum(large, half - 1)
    return side + np.where(n < max_exact, n, large)


def _constants():
    half = 64
    inv = (10000.0 ** (-np.arange(half, dtype=np.float64) / half))
    ang = np.arange(S, dtype=np.float64)[None, :] * inv[:, None]
    cosT = np.concatenate([np.cos(ang), np.cos(ang)], axis=0).astype(np.float32)
    sinT = np.concatenate([np.sin(ang), -np.sin(ang)], axis=0).astype(np.float32)
    j = np.arange(128)[:, None]
    i = np.arange(128)[None, :]
    p1 = np.maximum(i - j, 0).astype(np.float32)
    p2 = np.maximum(j - i, 0).astype(np.float32)
    eye = np.eye(128, dtype=np.float32)
    posf = np.broadcast_to((np.arange(128) + 1).astype(np.float32)[None, :], (128, 128))
    posb = np.broadcast_to((128 - np.arange(128)).astype(np.float32)[None, :], (128, 128))
    pcol = np.stack([127 - np.arange(128), np.arange(128)], axis=1).astype(np.float32)
    cst = np.concatenate([p1, p2, eye, posf, posb, pcol], axis=1).astype(np.float32)
    return cosT, sinT, np.ascontiguousarray(cst)


def _bias_tables(rel_bias):
    ii = np.arange(128)[:, None]
    jj = np.arange(256)[None, :]
    delta = ii + 64 - jj
    inwin = np.abs(delta) <= 64
    tb = np.full((128, 24, 256), NEG, dtype=np.float32)
    for pi, d in enumerate(PATTERNS):
        bk = _t5_bucket(delta * d)
        for h in range(8):
            sel = rel_bias[bk, h]
            tb[:, pi * 8 + h, :] = np.where(inwin, sel, np.float32(NEG))
    return tb.reshape(128, 24 * 256)


_NC_CACHE = {}


def kernel(x, w_in, ret_decay_fwd, ret_decay_bwd, ret_gn_g, w_proj_ret, rel_bias, w_proj_attn, w_out,
           ln1_g, ln1_b, router_w_group, router_b_group, router_w_expert, router_b_expert,
           w_gate, w_up, w_down, ln2_g, ln2_b):
    f = lambda a: np.ascontiguousarray(np.asarray(a, dtype=np.float32))
    x = f(x)
    cosT, sinT, cst = _constants()
    rep = lambda v: np.ascontiguousarray(np.broadcast_to(f(v).reshape(1, -1), (128, f(v).size)))
    shared = {
        "w_in": f(w_in)[0], "w_proj_ret": f(w_proj_ret)[0], "w_proj_attn": f(w_proj_attn)[0], "w_out": f(w_out)[0],
        "w_gate": f(w_gate)[0], "w_up": f(w_up)[0], "w_down": f(w_down)[0],
        "dec": rep(np.concatenate([f(ret_decay_fwd)[0], f(ret_decay_bwd)[0]])),
        "gng": np.ascontiguousarray(f(ret_gn_g)[0].reshape(8, 128).T), "ln1g": rep(f(ln1_g)[0]), "ln1b": rep(f(ln1_b)[0]),
        "ln2g": rep(f(ln2_g)[0]), "ln2b": rep(f(ln2_b)[0]),
        "rw": np.ascontiguousarray(np.concatenate([f(router_w_group)[0], f(router_w_expert)[0]], axis=1)),
        "rb": rep(np.concatenate([f(router_b_group)[0], f(router_b_expert)[0]])),
        "tb": _bias_tables(f(rel_bias)),
        "cosT": cosT, "sinT": sinT, "cst": cst,
    }
    in_maps = []
    for c in range(NCORES):
        xs = x[NSEQ * c:NSEQ * (c + 1)]
        m = dict(shared)
        m["x"] = np.ascontiguousarray(xs)
        m["xT"] = np.ascontiguousarray(xs.transpose(0, 2, 1))
        in_maps.append(m)
    if "nc" not in _NC_CACHE:
        _NC_CACHE["nc"] = build_program()
    res = run_bass_kernel_spmd(_NC_CACHE["nc"], in_maps, core_ids=list(range(NCORES)))
    if DBG:
        LAST["res"] = res.results
    return np.concatenate([np.asarray(r["out"], dtype=np.float32) for r in res.results], axis=0)
```
